# Optimizing a Trainium2 kernel written in Bass

```python
import jax, jax.numpy as jnp
from jax import lax
import numpy as np


D_MODEL = 1024
BATCH = 4
SEQ = 8192
DEPTH = 2

CHUNK = 64
Q_BLOCK = 128
P_DIM = 256
EPS = 1e-6
NEG_INF = -1e30
ROPE_THETA = 10000.0

MIX_WIDTH = D_MODEL
MLA_WIDTH = MIX_WIDTH // 2
FOX_WIDTH = MIX_WIDTH - MLA_WIDTH
MLA_V_DIM = 64
MLA_HEADS = MLA_WIDTH // MLA_V_DIM
MLA_NOPE_DIM = 64
MLA_ROPE_DIM = 32
MLA_QK_DIM = MLA_NOPE_DIM + MLA_ROPE_DIM
Q_LORA = D_MODEL // 4
KV_LORA = D_MODEL // 8
FOX_HEAD_DIM = 64
FOX_HEADS = FOX_WIDTH // FOX_HEAD_DIM

IN_SIZES = (Q_LORA, KV_LORA, MLA_ROPE_DIM, FOX_WIDTH, FOX_WIDTH, FOX_WIDTH, FOX_HEADS)
IN_COLS = sum(IN_SIZES)
IN_SPLITS = tuple(int(v) for v in np.cumsum(IN_SIZES)[:-1])

N_GROUPS = 4
EXPERTS_PER_GROUP = 8
N_EXPERTS = N_GROUPS * EXPERTS_PER_GROUP
TOP_K = 2
EXPERT_FF = D_MODEL // 4
EXPERT_BLOCK = 128

kernel_name = 'hybrid_mla_fox_hmoe_ple'


def rmsnorm(x, g):
    xf = x.astype(jnp.float32)
    y = xf * lax.rsqrt(jnp.mean(xf * xf, axis=-1, keepdims=True) + EPS)
    return (y * g.astype(jnp.float32)).astype(x.dtype)


def rope_tables(positions):
    inv_freq = ROPE_THETA ** (-jnp.arange(0, MLA_ROPE_DIM, 2, dtype=jnp.float32) / MLA_ROPE_DIM)
    ang = positions.astype(jnp.float32)[..., None] * inv_freq
    return jnp.cos(ang)[:, :, None, :], jnp.sin(ang)[:, :, None, :]


def apply_rope(x, cos, sin):
    half = MLA_ROPE_DIM // 2
    x1, x2 = x[..., :half], x[..., half:]
    out = jnp.concatenate([x1 * cos - x2 * sin, x2 * cos + x1 * sin], axis=-1)
    return out.astype(x.dtype)


def chunk_causal(q_idx, k_idx):
    return (k_idx // CHUNK) <= (q_idx // CHUNK)


def frame_causal(q_idx, k_idx):
    return k_idx <= q_idx


def blocked_attention(q, k, v, allowed, log_decay_cum=None):
    B, H, S, Dk = q.shape
    Dv = v.shape[-1]
    n_blocks = S // Q_BLOCK
    scale = Dk ** -0.5
    k_idx = jnp.arange(S, dtype=jnp.int32)

    def one_block(i):
        start = i * Q_BLOCK
        qb = lax.dynamic_slice_in_dim(q, start, Q_BLOCK, axis=2)
        q_idx = start + jnp.arange(Q_BLOCK, dtype=jnp.int32)
        s = jnp.einsum('bhqd,bhkd->bhqk', qb, k, preferred_element_type=jnp.float32) * scale
        if log_decay_cum is not None:
            f_q = lax.dynamic_slice_in_dim(log_decay_cum, start, Q_BLOCK, axis=2)
            s = s + f_q[..., :, None] - log_decay_cum[..., None, :]
        mask = allowed(q_idx[:, None], k_idx[None, :])
        s = jnp.where(mask, s, NEG_INF)
        w = jax.nn.softmax(s, axis=-1).astype(v.dtype)
        return jnp.einsum('bhqk,bhkd->bhqd', w, v)

    out = lax.map(one_block, jnp.arange(n_blocks, dtype=jnp.int32))
    return out.transpose(1, 2, 0, 3, 4).reshape(B, H, S, Dv)


def hybrid_mixer(h, cos, sin, w_in, g_q_lora, w_uq, g_kv_lora, w_ukv, g_mla_q, g_mla_k,
                 g_fox_q, g_fox_k, b_fox_f, g_out_mla, g_out_fox, w_out):
    B, S, _ = h.shape
    proj = h @ w_in
    q_c, kv_c, k_pe, fq, fk, fv, f_logit = jnp.split(proj, IN_SPLITS, axis=-1)

    q = (rmsnorm(q_c, g_q_lora) @ w_uq).reshape(B, S, MLA_HEADS, MLA_QK_DIM)
    kv = (rmsnorm(kv_c, g_kv_lora) @ w_ukv).reshape(B, S, MLA_HEADS, MLA_NOPE_DIM + MLA_V_DIM)
    k_nope, v_mla = kv[..., :MLA_NOPE_DIM], kv[..., MLA_NOPE_DIM:]
    k_pe_b = jnp.broadcast_to(k_pe[:, :, None, :], (B, S, MLA_HEADS, MLA_ROPE_DIM))
    k = jnp.concatenate([k_nope, k_pe_b], axis=-1)
    q = rmsnorm(q, g_mla_q)
    k = rmsnorm(k, g_mla_k)
    q = jnp.concatenate([q[..., :MLA_NOPE_DIM], apply_rope(q[..., MLA_NOPE_DIM:], cos, sin)], axis=-1)
    k = jnp.concatenate([k[..., :MLA_NOPE_DIM], apply_rope(k[..., MLA_NOPE_DIM:], cos, sin)], axis=-1)
    o_mla = blocked_attention(q.transpose(0, 2, 1, 3), k.transpose(0, 2, 1, 3),
                              v_mla.transpose(0, 2, 1, 3), chunk_causal)
    o_mla = o_mla.transpose(0, 2, 1, 3).reshape(B, S, MLA_WIDTH)

    fq = rmsnorm(fq.reshape(B, S, FOX_HEADS, FOX_HEAD_DIM), g_fox_q)
    fk = rmsnorm(fk.reshape(B, S, FOX_HEADS, FOX_HEAD_DIM), g_fox_k)
    fv = fv.reshape(B, S, FOX_HEADS, FOX_HEAD_DIM)
    log_f = jax.nn.log_sigmoid(f_logit.astype(jnp.float32) + b_fox_f.astype(jnp.float32))
    f_cum = jnp.cumsum(log_f, axis=1).transpose(0, 2, 1)
    o_fox = blocked_attention(fq.transpose(0, 2, 1, 3), fk.transpose(0, 2, 1, 3),
                              fv.transpose(0, 2, 1, 3), frame_causal, f_cum)
    o_fox = o_fox.transpose(0, 2, 1, 3).reshape(B, S, FOX_WIDTH)

    y = jnp.concatenate([rmsnorm(o_mla, g_out_mla), rmsnorm(o_fox, g_out_fox)], axis=-1)
    return y @ w_out


def hierarchical_moe(h, w_router_group, w_router_expert, w_exp_gate, w_exp_up, w_exp_down):
    B, S, D = h.shape
    N = B * S
    hf = h.reshape(N, D)
    p_group = jax.nn.softmax((hf @ w_router_group).astype(jnp.float32), axis=-1)
    g_idx = jnp.argmax(p_group, axis=-1).astype(jnp.int32)
    p_g = jnp.max(p_group, axis=-1)
    e_logits = (hf @ w_router_expert).astype(jnp.float32).reshape(N, N_GROUPS, EXPERTS_PER_GROUP)
    e_logits = jnp.take_along_axis(e_logits, g_idx[:, None, None], axis=1)[:, 0]
    p_exp = jax.nn.softmax(e_logits, axis=-1)
    top_p, top_i = lax.top_k(p_exp, TOP_K)
    gate = p_g[:, None] * top_p / jnp.sum(top_p, axis=-1, keepdims=True)
    expert_id = g_idx[:, None] * EXPERTS_PER_GROUP + top_i.astype(jnp.int32)

    A = N * TOP_K
    flat_e = expert_id.reshape(A)
    flat_g = gate.reshape(A)
    flat_tok = jnp.repeat(jnp.arange(N, dtype=jnp.int32), TOP_K)
    order = jnp.argsort(flat_e)
    e_sorted = flat_e[order]
    counts = jnp.zeros((N_EXPERTS,), jnp.int32).at[flat_e].add(1)
    padded = ((counts + EXPERT_BLOCK - 1) // EXPERT_BLOCK) * EXPERT_BLOCK
    pad_end = jnp.cumsum(padded)
    pad_start = pad_end - padded
    start = jnp.cumsum(counts) - counts
    dest = pad_start[e_sorted] + (jnp.arange(A, dtype=jnp.int32) - start[e_sorted])
    P = A + N_EXPERTS * EXPERT_BLOCK
    slot_tok = jnp.full((P,), N, jnp.int32).at[dest].set(flat_tok[order])
    slot_gate = jnp.zeros((P,), h.dtype).at[dest].set(flat_g[order].astype(h.dtype))
    n_blk = P // EXPERT_BLOCK
    blk_start = jnp.arange(n_blk, dtype=jnp.int32) * EXPERT_BLOCK
    blk_e = jnp.minimum(jnp.searchsorted(pad_end, blk_start, side='right'), N_EXPERTS - 1)

    h_pad = jnp.concatenate([hf, jnp.zeros((1, D), hf.dtype)], axis=0)
    xs = h_pad[slot_tok].reshape(n_blk, EXPERT_BLOCK, D)

    def expert_block(args):
        xb, e = args
        return (jax.nn.silu(xb @ w_exp_gate[e]) * (xb @ w_exp_up[e])) @ w_exp_down[e]

    ys = lax.map(expert_block, (xs, blk_e)).reshape(P, D)
    out = jnp.zeros((N + 1, D), h.dtype).at[slot_tok].add(ys * slot_gate[:, None])
    return out[:N].reshape(B, S, D)


def setup_inputs(seed: int = 0) -> dict:
    key = jax.random.key(seed)
    ks = jax.random.split(key, 27)
    f32 = jnp.float32

    def nrm(k, shape, fan_in):
        return jax.random.normal(k, shape, f32) * (fan_in ** -0.5)

    def gain(k, dim):
        return 1.0 + 0.05 * jax.random.normal(k, (DEPTH, dim), f32)

    x = jax.random.normal(ks[0], (BATCH, SEQ, D_MODEL), f32)
    p = jax.random.normal(ks[1], (DEPTH, BATCH, SEQ, P_DIM), f32)
    offsets = jax.random.randint(ks[2], (BATCH,), 0, 64, dtype=jnp.int32) * CHUNK
    positions = offsets[:, None] + jnp.arange(SEQ, dtype=jnp.int32)[None, :]
    return {
        'x': x,
        'p': p,
        'positions': positions,
        'g_attn_norm': gain(ks[3], D_MODEL),
        'w_in': nrm(ks[4], (DEPTH, D_MODEL, IN_COLS), D_MODEL),
        'g_q_lora': gain(ks[5], Q_LORA),
        'w_uq': nrm(ks[6], (DEPTH, Q_LORA, MLA_HEADS * MLA_QK_DIM), Q_LORA),
        'g_kv_lora': gain(ks[7], KV_LORA),
        'w_ukv': nrm(ks[8], (DEPTH, KV_LORA, MLA_HEADS * (MLA_NOPE_DIM + MLA_V_DIM)), KV_LORA),
        'g_mla_q': gain(ks[9], MLA_QK_DIM),
        'g_mla_k': gain(ks[10], MLA_QK_DIM),
        'g_fox_q': gain(ks[11], FOX_HEAD_DIM),
        'g_fox_k': gain(ks[12], FOX_HEAD_DIM),
        'b_fox_f': 2.0 + 0.5 * jax.random.normal(ks[13], (DEPTH, FOX_HEADS), f32),
        'g_out_mla': gain(ks[14], MLA_WIDTH),
        'g_out_fox': gain(ks[15], FOX_WIDTH),
        'w_out': nrm(ks[16], (DEPTH, MIX_WIDTH, D_MODEL), MIX_WIDTH),
        'g_ffn_norm': gain(ks[17], D_MODEL),
        'w_router_group': nrm(ks[18], (DEPTH, D_MODEL, N_GROUPS), D_MODEL),
        'w_router_expert': nrm(ks[19], (DEPTH, D_MODEL, N_EXPERTS), D_MODEL),
        'w_exp_gate': nrm(ks[20], (DEPTH, N_EXPERTS, D_MODEL, EXPERT_FF), D_MODEL),
        'w_exp_up': nrm(ks[21], (DEPTH, N_EXPERTS, D_MODEL, EXPERT_FF), D_MODEL),
        'w_exp_down': nrm(ks[22], (DEPTH, N_EXPERTS, EXPERT_FF, D_MODEL), EXPERT_FF),
        'g_ple_norm': gain(ks[23], D_MODEL),
        'w_ple_gate': nrm(ks[24], (DEPTH, D_MODEL, D_MODEL), D_MODEL),
        'w_ple_proj': nrm(ks[25], (DEPTH, P_DIM, D_MODEL), P_DIM),
        'g_ple_out': gain(ks[26], D_MODEL),
    }


def reference(x, p, positions, g_attn_norm, w_in, g_q_lora, w_uq, g_kv_lora, w_ukv, g_mla_q, g_mla_k,
              g_fox_q, g_fox_k, b_fox_f, g_out_mla, g_out_fox, w_out, g_ffn_norm, w_router_group,
              w_router_expert, w_exp_gate, w_exp_up, w_exp_down, g_ple_norm, w_ple_gate, w_ple_proj,
              g_ple_out):
    cos, sin = rope_tables(positions)
    for i in range(DEPTH):
        h = rmsnorm(x, g_attn_norm[i])
        x = x + hybrid_mixer(h, cos, sin, w_in[i], g_q_lora[i], w_uq[i], g_kv_lora[i], w_ukv[i],
                             g_mla_q[i], g_mla_k[i], g_fox_q[i], g_fox_k[i], b_fox_f[i],
                             g_out_mla[i], g_out_fox[i], w_out[i])
        h = rmsnorm(x, g_ffn_norm[i])
        x = x + hierarchical_moe(h, w_router_group[i], w_router_expert[i], w_exp_gate[i],
                                 w_exp_up[i], w_exp_down[i])
        ple = rmsnorm(p[i] @ w_ple_proj[i], g_ple_out[i])
        ple_gate = jax.nn.sigmoid(rmsnorm(x, g_ple_norm[i]) @ w_ple_gate[i])
        x = x + ple * ple_gate
    return x
```

```python
import contextlib
import os
import numpy as np
import concourse.bass as bass
import concourse.mybir as mybir
from concourse.bass_utils import run_bass_kernel_spmd

F32 = mybir.dt.float32
BF16 = mybir.dt.bfloat16
I32 = mybir.dt.int32
ALU = mybir.AluOpType
AF = mybir.ActivationFunctionType
AX = mybir.AxisListType

NT = 8192
TT = 512
NTT = NT // TT
EPS = 1e-6
NEG = -30000.0
M0 = 12.0
BIG = 1.0e9

COMPUTE = ("pe", "act", "dve", "pool")
ALLENG = ("pe", "act", "dve", "pool", "sp")


class Res:
    __slots__ = ("name", "w", "r", "psum")

    def __init__(self, name="", psum=False):
        self.name = name
        self.w = None
        self.r = []
        self.psum = psum


class Prog:
    def __init__(self, nc, stack):
        self.nc = nc
        self.stack = stack
        self.ops = {e: [] for e in ALLENG}
        self.cnt = {}
        self.sem = {}
        self.seen = {e: {} for e in ALLENG}
        self.hook = None
        for e in COMPUTE:
            self._mk_sem(e)

    def _mk_sem(self, key):
        self.sem[key] = self.stack.enter_context(self.nc.semaphore("s_" + str(key)))
        self.cnt[key] = 0

    def dsem(self, name):
        key = "d_" + name
        if key not in self.sem:
            self._mk_sem(key)
        return key

    def sb(self, name, shape, dt):
        return self.stack.enter_context(self.nc.sbuf_tensor(name, list(shape), dt))

    def ps(self, name, shape, dt=F32):
        return self.stack.enter_context(self.nc.psum_tensor(name, list(shape), dt))

    def _deps(self, eng, reads, writes):
        deps = []
        for r in reads:
            if r.w is not None:
                deps.append(r.w)
            if r.psum:
                deps.extend(ev for ev in r.r if ev[2] != eng)
        for w in writes:
            if w.w is not None:
                if not (eng == "pe" and w.w[2] == "pe"):
                    deps.append(w.w)
            deps.extend(w.r)
        waits = {}
        for (k, v, e) in deps:
            if self.seen[eng].get(k, 0) >= v:
                continue
            if waits.get(k, 0) < v:
                waits[k] = v
        for k, v in waits.items():
            self.seen[eng][k] = v
        return list(waits.items())

    def _commit(self, ev, reads, writes):
        for r in reads:
            r.r.append(ev)
        for w in writes:
            w.w = ev
            w.r = []

    def op(self, eng, fn, reads=(), writes=()):
        waits = self._deps(eng, reads, writes)
        self.cnt[eng] += 1
        ev = (eng, self.cnt[eng], eng)
        self.ops[eng].append((waits, fn, (eng, 1)))
        self._commit(ev, reads, writes)
        if self.hook:
            self.hook()

    def dma(self, q, out, in_, sem, reads=(), writes=(), slow=False):
        waits = self._deps(q, reads, writes)
        self.cnt[sem] += 16
        ev = (sem, self.cnt[sem], "dma")
        if slow:
            self.ops[q].append((waits, lambda e: e.dma_start(out=out, in_=in_, allow_slow_non_contiguous=True), (sem, 16)))
        else:
            self.ops[q].append((waits, lambda e: e.dma_start(out=out, in_=in_), (sem, 16)))
        self._commit(ev, reads, writes)
        if self.hook:
            self.hook()

    def barrier(self):
        for e in ALLENG:
            waits = []
            for k, v in self.cnt.items():
                if v > self.seen[e].get(k, 0):
                    waits.append((k, v))
                    self.seen[e][k] = v
            self.ops[e].append((waits, None, None))

    def mm(self, out, lhsT, rhs, start, stop, reads, writes):
        self.op("pe", lambda e: e.matmul(out, lhsT=lhsT, rhs=rhs, start=start, stop=stop), reads, writes)

    def act(self, out, in_, func, reads, writes, bias=None, scale=1.0):
        if bias is None:
            self.op("act", lambda e: e.activation(out=out, in_=in_, func=func, scale=scale), reads, writes)
        else:
            self.op("act", lambda e: e.activation(out=out, in_=in_, func=func, bias=bias, scale=scale), reads, writes)

    def stt(self, out, in0, scalar, in1, op0, op1, reads, writes, eng="dve"):
        self.op(eng, lambda e: e.scalar_tensor_tensor(out=out, in0=in0, scalar=scalar, in1=in1, op0=op0, op1=op1),
                reads, writes)

    def tt(self, out, in0, in1, op, reads, writes, eng="dve"):
        self.op(eng, lambda e: e.tensor_tensor(out=out, in0=in0, in1=in1, op=op), reads, writes)

    def ts(self, out, in0, s1, op0, reads, writes, s2=None, op1=None, eng="dve"):
        if op1 is None:
            self.op(eng, lambda e: e.tensor_single_scalar(out=out, in_=in0, scalar=s1, op=op0), reads, writes)
        else:
            self.op(eng, lambda e: e.tensor_scalar(out=out, in0=in0, scalar1=s1, scalar2=s2, op0=op0, op1=op1),
                    reads, writes)

    def cp(self, out, in_, reads, writes, eng="dve"):
        self.op(eng, lambda e: e.tensor_copy(out=out, in_=in_), reads, writes)

    def emit(self):
        nc = self.nc
        handles = {"pe": "tensor", "act": "scalar", "dve": "vector", "pool": "gpsimd", "sp": "sync"}
        with nc.Block() as block:
            for en in ALLENG:
                ops = self.ops[en]

                def body(eng, ops=ops):
                    for waits, fn, inc in ops:
                        for k, v in waits:
                            eng.wait_ge(self.sem[k], v)
                        if fn is None:
                            continue
                        fn(eng).then_inc(self.sem[inc[0]], inc[1])
                getattr(block, handles[en])(body)


def interleave(P, f0, f1, every=4):
    import threading
    sems = [threading.Semaphore(0), threading.Semaphore(0)]
    done = [False, False]
    cnt = [0]
    cur = [0]
    err = []

    def hook():
        cnt[0] += 1
        if cnt[0] >= every:
            cnt[0] = 0
            me = cur[0]
            other = 1 - me
            if not done[other]:
                cur[0] = other
                sems[other].release()
                sems[me].acquire()

    def run(i, f):
        sems[i].acquire()
        try:
            f()
        except BaseException as ex:
            err.append(ex)
        finally:
            done[i] = True
            other = 1 - i
            if not done[other]:
                cur[0] = other
                sems[other].release()

    th = [threading.Thread(target=run, args=(i, f)) for i, f in enumerate((f0, f1))]
    P.hook = hook
    for t in th:
        t.start()
    sems[0].release()
    for t in th:
        t.join()
    P.hook = None
    if err:
        raise err[0]


class Ring:
    def __init__(self, P, name, shape, dt, n, psum=False):
        self.t = []
        self.r = []
        for i in range(n):
            nm = f"{name}{i}"
            self.t.append(P.ps(nm, shape, dt) if psum else P.sb(nm, shape, dt))
            self.r.append(Res(nm))
        self.sems = [P.dsem(f"{name}{i}") for i in range(n)] if not psum else None
        self.i = 0
        self.n = n

    def next(self):
        i = self.i
        self.i = (i + 1) % self.n
        if self.sems:
            return self.t[i], self.r[i], self.sems[i]
        return self.t[i], self.r[i]


PC = dict(g_attn=0, g_ffn=8, g_plen=16, g_pleo=24, g_out=32, g_ql=40, g_kvl=42, g_mq=43, g_mk=44,
          g_fq=45, g_fk=46, b_f=47)
NP = 48
CC = dict(ident=0, ones=128, bd64=256, rot=384, eeven=512, eodd=640, mfr=768, mch=768 + 2048, freq=768 + 4096)
NCST = 768 + 4096 + 1


def build_program(dbg=None):
    nc = bass.Bass("TRN2", target_bir_lowering=False)
    stop = (dbg or {}).get("stop")
    douts = (dbg or {}).get("outs", [])

    def din(name, shape, dt=F32):
        return nc.dram_tensor(name, list(shape), dt, kind="ExternalInput").ap()

    def dint(name, shape, dt):
        kind = "ExternalOutput" if name in douts else "Internal"
        return nc.dram_tensor(name, list(shape), dt, kind=kind).ap()

    xT = din("xT", [1024, NT])
    pT = din("pT", [2, 256, NT])
    pos = din("pos", [1, NT], I32)
    prm_d = din("prm", [2, 128, NP])
    cst_d = din("cst", [128, NCST])
    w_in = din("w_in", [2, 1024, 1960])
    w_uq = din("w_uq", [2, 256, 768])
    w_ukv = din("w_ukv", [2, 128, 1024])
    w_out = din("w_out", [2, 1024, 1024])
    w_rt = din("w_rt", [2, 1024, 36])
    NE_ = 1 if os.environ.get("KSMALL") else 32
    w_eg = din("w_eg", [2, NE_, 1024, 256])
    w_eu = din("w_eu", [2, NE_, 1024, 256])
    w_ed = din("w_ed", [2, NE_, 256, 1024])
    w_pg = din("w_pg", [2, 1024, 1024])
    w_pp = din("w_pp", [2, 256, 1024])
    yT = nc.dram_tensor("yT", [1024, NT], F32, kind="ExternalOutput").ap()

    xa = dint("xa", [1024, NT], F32)
    xb = dint("xb", [1024, NT], F32)
    xc = dint("xc", [1024, NT], F32)
    ropeT = dint("ropeT", [2, 96, NT], F32)
    qm = dint("qm", [8, 96, NT], BF16)
    km = dint("km", [8, 96, NT], BF16)
    vm = dint("vm", [NT, 512], BF16)
    qf = dint("qf", [4, 128, NT], BF16)
    kf = dint("kf", [4, 128, NT], BF16)
    vf = dint("vf", [NT, 512], BF16)
    lfd = dint("lfd", [8, NT], F32)
    kaug = dint("kaug", [8, 4, NT], BF16)
    qaug = dint("qaug", [8, 4, NT], BF16)
    omla = dint("omla", [4, 128, NT], BF16)
    ofox = dint("ofox", [4, 128, NT], BF16)
    h2d = dint("h2d", [1024, NT], BF16)
    gTd = dint("gTd", [32, NT], F32)

    with contextlib.ExitStack() as st:
        P = Prog(nc, st)
        BA = P.sb("BA", [128, 53504], BF16)
        FA = P.sb("FA", [128, 22528], F32)
        PSA = P.ps("PSA", [128, 2048], F32)
        PSB = P.ps("PSB", [128, 2048], F32)
        banks = [PSA[:, i * 512:(i + 1) * 512] for i in range(4)] + [PSB[:, i * 512:(i + 1) * 512] for i in range(4)]
        bres = [Res(f"bank{i}", psum=True) for i in range(8)]
        cst = P.sb("cst_sb", [128, 768], BF16)
        cstf = P.sb("cstf", [128, 257], F32)
        msk = P.sb("msk", [128, 4096], BF16)
        prm = P.sb("prm_sb", [128, 2 * NP], F32)
        negm = P.sb("negm", [128, 1], F32)
        kc = P.sb("kc", [128, 2], F32)
        Rc = Res("const")
        sem_c = P.dsem("const")
        sem_cp = P.dsem("const_swdge")
        P.dma("pool", cst[:, 0:768], cst_d[:, 0:768], sem_cp, writes=[Rc])
        P.dma("pool", msk[:, :], cst_d[:, 768:768 + 4096], sem_cp, writes=[Rc])
        P.dma("sp", cstf[:, 0:256], cst_d[:, 512:768], sem_c, writes=[Rc])
        P.dma("sp", cstf[:, 256:257], cst_d[:, 768 + 4096:768 + 4097], sem_c, writes=[Rc], slow=True)
        P.dma("sp", prm[:, 0:NP], prm_d[0], sem_c, writes=[Rc])
        P.dma("sp", prm[:, NP:2 * NP], prm_d[1], sem_c, writes=[Rc])
        P.op("dve", lambda e: e.memset(negm[:], -M0), writes=[Rc])
        P.op("dve", lambda e: e.memset(kc[:, 0:1], EPS), writes=[Rc])
        P.op("dve", lambda e: e.memset(kc[:, 1:2], 1.0), writes=[Rc])
        ident = cst[:, 0:128]
        ones = cst[:, 128:256]
        bd64 = cst[:, 256:384]
        rot = cst[0:96, 384:480]
        eev_f = cstf[:, 0:128]
        eod_f = cstf[:, 128:256]
        freq = cstf[0:96, 256:257]

        def pcol(l, name, c=0, rows=slice(0, 128)):
            j = l * NP + PC[name] + c
            return prm[rows, j:j + 1]

        for l in range(2):
            P.ts(pcol(l, "g_mq"), pcol(l, "g_mq"), float(96 ** -0.5), ALU.mult, [Rc], [Rc])
            P.ts(pcol(l, "g_fq"), pcol(l, "g_fq"), float(64 ** -0.5), ALU.mult, [Rc], [Rc])
        P.barrier()

        def rstd_from_ps(psb, rows, n, out, rd, wr, tmp):
            P.act(tmp, psb, AF.Ln, list(rd) + [Rc], wr, bias=kc[0:rows, 0:1], scale=1.0 / n)
            P.act(out, tmp, AF.Exp, wr, wr, scale=-0.5)

        def phase_rope():
            posi = FA[0:96, 0:512].bitcast(I32)
            posf = FA[0:96, 512:1024]
            ang = FA[0:96, 1024:1536]
            cs = FA[0:96, 1536:2048]
            sn = FA[0:96, 2048:2560]
            ki = FA[0:96, 2560:3072].bitcast(I32)
            kf = FA[0:96, 3072:3584]
            R = Res("rope")
            sl = P.dsem("rope_l")
            ss_ = P.dsem("rope_s")
            for t in range(NTT):
                ts_ = slice(t * TT, (t + 1) * TT)
                P.dma("sp", posi, pos[0:1, ts_].partition_broadcast(96), sl, writes=[R])
                P.cp(posf, posi, [R], [R])
                P.ts(ang, posf, freq, ALU.mult, [R, Rc], [R])
                for (dst, off) in ((cs, 0.25), (sn, 0.0)):
                    P.ts(posf, ang, float(1.0 / (2 * np.pi)), ALU.mult, [R], [R], s2=float(off), op1=ALU.add)
                    P.cp(ki, posf, [R], [R])
                    P.cp(kf, ki, [R], [R])
                    P.tt(posf, posf, kf, ALU.subtract, [R], [R])
                    P.ts(kf, posf, 0.5, ALU.is_gt, [R], [R])
                    P.tt(posf, posf, kf, ALU.subtract, [R], [R])
                    P.act(dst, posf, AF.Sin, [R], [R], scale=float(2 * np.pi))
                P.dma("pool", ropeT[0, 64:96, ts_], cs[64:96, :], ss_, reads=[R])
                P.dma("pool", ropeT[1, 64:96, ts_], sn[64:96, :], ss_, reads=[R])
            P.barrier()

        def phase_A(l, xsrc):
            o = 0
            Win = BA[:, o:o + 8 * 1960].rearrange("p (c n) -> p c n", c=8); o += 8 * 1960
            Wkpe = BA[:, o:o + 8 * 96].rearrange("p (c n) -> p c n", c=8); o += 8 * 96
            Wuq = BA[:, o:o + 2 * 768].rearrange("p (c n) -> p c n", c=2); o += 2 * 768
            Wukv = BA[:, o:o + 1024]; o += 1024
            Wv = BA[:, o:o + 512].rearrange("p (h d) -> p h d", h=8); o += 512
            stg = [BA[:, o + i * 512:o + (i + 1) * 512] for i in range(6)]; o += 3072
            Rw = Res("wA"); Rstg = [Res(f"stg{i}") for i in range(6)]
            sw = P.dsem("wA")
            sst = [P.dsem(f"stg{i}") for i in range(6)]
            off = dict(o=o, f=0)

            def alloc(pi):
                o = off["o"]; f = off["f"]
                sq = BA[:, o:o + 8 * 512].rearrange("p (c n) -> p c n", c=8); o += 4096
                ht = BA[:, o:o + 8 * 512].rearrange("p (c n) -> p c n", c=8); o += 4096
                qcn = BA[:, o:o + 1024].rearrange("p (c n) -> p c n", c=2); o += 1024
                kvn = BA[:, o:o + 512]; o += 512
                sqs = [BA[:, o + i * 512:o + (i + 1) * 512] for i in range(2)]; o += 1024
                kpg = BA[:, o:o + 512]; o += 512
                vst = [BA[:, o + i * 2048:o + (i + 1) * 2048].rearrange("p (s n) -> p s n", s=4) for i in range(2)]; o += 4096
                assert o <= 53504
                x_t = FA[:, f:f + 4096].rearrange("p (c n) -> p c n", c=8); f += 4096
                rstd = FA[:, f:f + 512]; f += 512
                tmp = FA[:, f:f + 512]; f += 512
                qcr = FA[:, f:f + 1024].rearrange("p (c n) -> p c n", c=2); f += 1024
                rr = [FA[:, f + i * 512:f + (i + 1) * 512] for i in range(2)]; f += 1024
                t1 = FA[:, f:f + 512]; f += 512
                t2 = FA[:, f:f + 512]; f += 512
                kr = FA[:, f:f + 512]; f += 512
                cs = FA[:, f:f + 512]; f += 512
                sn = FA[:, f:f + 512]; f += 512
                lz = FA[:, f:f + 512]; f += 512
                la = FA[:, f:f + 512]; f += 512
                assert f <= 22528
                Rsq = Res("sq"); Rht = Res("ht"); Rqcn = Res("qcn"); Rkvn = Res("kvn")
                Rsqs = [Res("sqs0"), Res("sqs1")]; Rkpg = Res("kpg")
                Rvst = [Res("vst0"), Res("vst1")]; Rx = Res("xt"); Rrstd = Res("rstd"); Rtmp = Res("tmp")
                Rqcr = Res("qcr"); Rrr = [Res("rr0"), Res("rr1")]; Rt1 = Res("t1"); Rt2 = Res("t2"); Rkr = Res("kr")
                Rcs = Res("cs"); Rl = Res("lz")
                sxx = P.dsem(f"xt{pi}")
                scs = P.dsem(f"cs{pi}")
                svs = [P.dsem(f"vst0_{pi}"), P.dsem(f"vst1_{pi}")]
                sl = P.dsem(f"lz{pi}")
                P.op("dve", lambda e: e.memset(kpg, 0.0), writes=[Rkpg])
                off["o"] = o; off["f"] = f
                d = dict(locals())
                for k_ in ("o", "f", "pi"):
                    d.pop(k_)
                return d

            bufs = [alloc(0), alloc(1)]
            wv = w_in[l].rearrange("(c p) n -> p c n", p=128)
            for c in range(8):
                P.dma("pool", Win[:, c, :], wv[:, c, :], sw, writes=[Rw])
            P.dma("pool", Wuq, w_uq[l].rearrange("(c p) n -> p c n", p=128), sw, writes=[Rw])
            P.dma("pool", Wukv, w_ukv[l], sw, writes=[Rw])
            P.dma("pool", Wv, w_ukv[l].rearrange("p (h two d) -> p h two d", h=8, two=2)[:, :, 1, :], sw, writes=[Rw])
            P.op("dve", lambda e: e.memset(Wkpe, 0.0), writes=[Rw])
            P.cp(Wkpe[:, :, 64:96], Win[:, :, 384:416], [Rw], [Rw])
            stg_i = [0]

            def stage():
                i = stg_i[0]
                stg_i[0] = (i + 1) % 6
                return stg[i], Rstg[i], sst[i]

            bk = [0]

            def bank():
                i = bk[0]
                bk[0] = (i + 1) % 8
                return banks[i], bres[i]

            xv = xsrc.rearrange("(c p) t -> p c t", p=128)
            KS = int(os.environ.get('KA_STOP', '99'))

            def tile(t, sq, ht, qcn, kvn, sqs, kpg, vst, x_t, rstd, tmp, qcr, rr, t1, t2, kr, cs, sn, lz, la,
                     Rsq, Rht, Rqcn, Rkvn, Rsqs, Rkpg, Rvst, Rx, Rrstd, Rtmp, Rqcr, Rrr, Rt1, Rt2, Rkr, Rcs, Rl,
                     sxx, scs, svs, sl, **_):
                ts_ = slice(t * TT, (t + 1) * TT)
                for c in range(0, 8, 2):
                    P.dma("sp", x_t[:, c:c + 2, :], xv[:, c:c + 2, ts_], sxx, writes=[Rx])
                P.dma("sp", cs[64:96, :], ropeT[0, 64:96, ts_], scs, writes=[Rcs])
                P.dma("sp", sn[64:96, :], ropeT[1, 64:96, ts_], scs, writes=[Rcs])
                for c in range(8):
                    P.act(sq[:, c, :], x_t[:, c, :], AF.Square, [Rx], [Rsq])
                pb, Rb = bank()
                for c in range(8):
                    P.mm(pb, ones, sq[:, c, :], c == 0, c == 7, [Rsq, Rc], [Rb])
                rstd_from_ps(pb, 128, 1024.0, rstd, [Rb], [Rrstd], rstd)
                for c in range(8):
                    P.stt(ht[:, c, :], x_t[:, c, :], pcol(l, "g_attn", c), rstd, ALU.mult, ALU.mult,
                          [Rx, Rrstd, Rc], [Rht])
                if KS <= 1:
                    return
                pq = []
                for j in range(2):
                    pb, Rb = bank()
                    for c in range(8):
                        P.mm(pb, Win[:, c, j * 128:(j + 1) * 128], ht[:, c, :], c == 0, c == 7, [Rw, Rht], [Rb])
                    P.cp(qcr[:, j, :], pb, [Rb], [Rqcr])
                    P.act(sq[:, j, :], pb, AF.Square, [Rb], [Rsq])
                pb, Rb = bank()
                for j in range(2):
                    P.mm(pb, ones, sq[:, j, :], j == 0, j == 1, [Rsq, Rc], [Rb])
                rstd_from_ps(pb, 128, 256.0, tmp, [Rb], [Rtmp], tmp)
                for j in range(2):
                    P.stt(qcn[:, j, :], qcr[:, j, :], pcol(l, "g_ql", j), tmp, ALU.mult, ALU.mult,
                          [Rqcr, Rtmp, Rc], [Rqcn])
                if KS <= 2:
                    return
                pkv, Rkv = bank()
                for c in range(8):
                    P.mm(pkv, Win[:, c, 256:384], ht[:, c, :], c == 0, c == 7, [Rw, Rht], [Rkv])
                P.act(sq[:, 2, :], pkv, AF.Square, [Rkv], [Rsq])
                pb, Rb = bank()
                P.mm(pb, ones, sq[:, 2, :], True, True, [Rsq, Rc], [Rb])
                rstd_from_ps(pb, 128, 128.0, tmp, [Rb], [Rtmp], tmp)
                P.stt(kvn, pkv, pcol(l, "g_kvl"), tmp, ALU.mult, ALU.mult, [Rkv, Rtmp, Rc], [Rkvn])
                if KS <= 3:
                    return
                for h in range(8):
                    pb, Rb = bank()
                    for j in range(2):
                        P.mm(pb[0:96, :], Wuq[:, j, h * 96:(h + 1) * 96], qcn[:, j, :], j == 0, j == 1, [Rw, Rqcn], [Rb])
                    s_, Rs_ = sqs[h % 2], Rsqs[h % 2]
                    P.act(s_[0:96, :], pb[0:96, :], AF.Square, [Rb], [Rs_])
                    p2, R2 = bank()
                    P.mm(p2[0:96, :], ones[0:96, 0:96], s_[0:96, :], True, True, [Rs_, Rc], [R2])
                    r_, Rr_ = rr[h % 2], Rrr[h % 2]
                    rstd_from_ps(p2[0:96, :], 96, 96.0, r_[0:96, :], [R2], [Rr_], r_[0:96, :])
                    so, Rso, sso = stage()
                    P.stt(so[0:96, :], pb[0:96, :], pcol(l, "g_mq", 0, slice(0, 96)), r_[0:96, :], ALU.mult, ALU.mult,
                          [Rb, Rr_, Rc], [Rso])
                    p3, R3 = bank()
                    P.mm(p3[0:96, :], rot, so[0:96, :], True, True, [Rso, Rc], [R3])
                    P.tt(t1[64:96, :], so[64:96, :], cs[64:96, :], ALU.mult, [Rso, Rcs], [Rt1])
                    P.tt(t2[64:96, :], p3[64:96, :], sn[64:96, :], ALU.mult, [R3, Rcs], [Rt2])
                    P.tt(so[64:96, :], t1[64:96, :], t2[64:96, :], ALU.add, [Rt1, Rt2], [Rso])
                    P.dma("pool", qm[h, :, ts_], so[0:96, :], sso, reads=[Rso])
                if KS <= 4:
                    return
                pkp, Rkp = bank()
                for c in range(8):
                    P.mm(pkp[0:96, :], Wkpe[:, c, :], ht[:, c, :], c == 0, c == 7, [Rw, Rht], [Rkp])
                for i in range(2):
                    P.act(sqs[i][64:96, :], pkp[64:96, :], AF.Square, [Rkp], [Rsqs[i]])
                P.ts(kpg[64:96, :], pkp[64:96, :], pcol(l, "g_mk", 0, slice(64, 96)), ALU.mult, [Rkp, Rc], [Rkpg])
                p3, R3 = bank()
                P.mm(p3[0:96, :], rot, kpg[0:96, :], True, True, [Rkpg, Rc], [R3])
                P.tt(t1[64:96, :], kpg[64:96, :], cs[64:96, :], ALU.mult, [Rkpg, Rcs], [Rt1])
                P.tt(t2[64:96, :], p3[64:96, :], sn[64:96, :], ALU.mult, [R3, Rcs], [Rt2])
                P.tt(kr[64:96, :], t1[64:96, :], t2[64:96, :], ALU.add, [Rt1, Rt2], [Rkr])
                for h in range(8):
                    pb, Rb = bank()
                    P.mm(pb[0:64, :], Wukv[:, h * 128:h * 128 + 64], kvn, True, True, [Rw, Rkvn], [Rb])
                    s_, Rs_ = sqs[h % 2], Rsqs[h % 2]
                    P.act(s_[0:64, :], pb[0:64, :], AF.Square, [Rb], [Rs_])
                    p2, R2 = bank()
                    P.mm(p2[0:96, :], ones[0:96, 0:96], s_[0:96, :], True, True, [Rs_, Rc], [R2])
                    r_, Rr_ = rr[h % 2], Rrr[h % 2]
                    rstd_from_ps(p2[0:96, :], 96, 96.0, r_[0:96, :], [R2], [Rr_], r_[0:96, :])
                    so, Rso, sso = stage()
                    P.stt(so[0:64, :], pb[0:64, :], pcol(l, "g_mk", 0, slice(0, 64)), r_[0:64, :], ALU.mult, ALU.mult,
                          [Rb, Rr_, Rc], [Rso])
                    P.tt(so[64:96, :], kr[64:96, :], r_[64:96, :], ALU.mult, [Rkr, Rr_], [Rso])
                    P.dma("pool", km[h, :, ts_], so[0:96, :], sso, reads=[Rso])
                if KS <= 5:
                    return
                v_, Rv_, sv_ = vst[0], Rvst[0], svs[0]
                for s in range(4):
                    pb, Rb = bank()
                    P.mm(pb, kvn[:, s * 128:(s + 1) * 128], Wv.rearrange("p h d -> p (h d)"), True, True, [Rw, Rkvn], [Rb])
                    P.cp(v_[:, s, :], pb, [Rb], [Rv_])
                P.dma("pool", vm[ts_, :].rearrange("(s p) n -> p s n", p=128), v_, sv_, reads=[Rv_])
                if KS <= 6:
                    return
                for (dst, col0, gname) in ((qf, 416, "g_fq"), (kf, 928, "g_fk")):
                    for j in range(4):
                        pb, Rb = bank()
                        for c in range(8):
                            P.mm(pb, Win[:, c, col0 + j * 128:col0 + (j + 1) * 128], ht[:, c, :], c == 0, c == 7,
                                 [Rw, Rht], [Rb])
                        s_, Rs_ = sqs[j % 2], Rsqs[j % 2]
                        P.act(s_, pb, AF.Square, [Rb], [Rs_])
                        p2, R2 = bank()
                        P.mm(p2, bd64, s_, True, True, [Rs_, Rc], [R2])
                        r_, Rr_ = rr[j % 2], Rrr[j % 2]
                        rstd_from_ps(p2, 128, 64.0, r_, [R2], [Rr_], r_)
                        so, Rso, sso = stage()
                        P.stt(so, pb, pcol(l, gname), r_, ALU.mult, ALU.mult, [Rb, Rr_, Rc], [Rso])
                        P.dma("pool", dst[j, :, ts_], so, sso, reads=[Rso])
                if KS <= 7:
                    return
                v_, Rv_, sv_ = vst[1], Rvst[1], svs[1]
                for s in range(4):
                    pb, Rb = bank()
                    for c in range(8):
                        P.mm(pb, ht[:, c, s * 128:(s + 1) * 128], Win[:, c, 1440:1952], c == 0, c == 7, [Rw, Rht], [Rb])
                    P.cp(v_[:, s, :], pb, [Rb], [Rv_])
                P.dma("pool", vf[ts_, :].rearrange("(s p) n -> p s n", p=128), v_, sv_, reads=[Rv_])
                if KS <= 8:
                    return
                pb, Rb = bank()
                for c in range(8):
                    P.mm(pb[0:8, :], Win[:, c, 1952:1960], ht[:, c, :], c == 0, c == 7, [Rw, Rht], [Rb])
                P.ts(lz[0:8, :], pb[0:8, :], pcol(l, "b_f", 0, slice(0, 8)), ALU.add, [Rb, Rc], [Rl])
                P.ts(la[0:8, :], lz[0:8, :], -1.0, ALU.mult, [Rl], [Rl])
                P.tt(la[0:8, :], la[0:8, :], lz[0:8, :], ALU.max, [Rl], [Rl])
                P.act(la[0:8, :], la[0:8, :], AF.Exp, [Rl], [Rl], scale=-1.0)
                P.act(la[0:8, :], la[0:8, :], AF.Ln, [Rl, Rc], [Rl], bias=kc[0:8, 1:2])
                P.ts(lz[0:8, :], lz[0:8, :], 0.0, ALU.min, [Rl], [Rl])
                P.tt(lz[0:8, :], lz[0:8, :], la[0:8, :], ALU.subtract, [Rl], [Rl])
                P.dma("pool", lfd[:, ts_], lz[0:8, :], sl, reads=[Rl])
            for t in range(0, int(os.environ.get('KA_TILES', NTT)), 2):
                interleave(P, lambda: tile(t, **bufs[0]), lambda: tile(t + 1, **bufs[1]))
            P.barrier()

        def phase_scan(l):
            CH = 2048
            lf = [FA[0:8, i * CH:(i + 1) * CH] for i in range(2)]
            fc = [FA[0:8, (2 + i) * CH:(3 + i) * CH] for i in range(2)]
            r1 = FA[0:8, 4 * CH:5 * CH]
            r2 = FA[0:8, 5 * CH:6 * CH]
            onesf = FA[0:8, 6 * CH:7 * CH]
            hb = [BA[0:8, i * CH:(i + 1) * CH] for i in range(4)]
            oneb = BA[0:8, 4 * CH:5 * CH]
            R = Res("scan"); Rlf = [Res("lf0"), Res("lf1")]; Rfc = [Res("fc0"), Res("fc1")]
            Rhb = Res("hb")
            s_l = [P.dsem("lf0"), P.dsem("lf1")]
            s_s = P.dsem("scan_s")
            P.op("dve", lambda e: e.memset(onesf, 1.0), writes=[R])
            P.op("dve", lambda e: e.memset(oneb, 1.0), writes=[R])
            for c in range(NT // CH):
                cs_ = slice(c * CH, (c + 1) * CH)
                l_, Rl_ = lf[c % 2], Rlf[c % 2]
                f_, Rf_ = fc[c % 2], Rfc[c % 2]
                P.dma("sp", l_, lfd[:, cs_], s_l[c % 2], writes=[Rl_])
                if c == 0:
                    P.op("dve", lambda e, f_=f_, l_=l_: e.tensor_tensor_scan(out=f_, data0=onesf, data1=l_, initial=0.0,
                                                                             op0=ALU.mult, op1=ALU.add),
                         [R, Rl_], [Rf_])
                else:
                    pf = fc[(c - 1) % 2]
                    P.op("dve", lambda e, f_=f_, l_=l_, pf=pf: e.tensor_tensor_scan(
                        out=f_, data0=onesf, data1=l_, initial=pf[:, CH - 1:CH], op0=ALU.mult, op1=ALU.add),
                        [R, Rl_, Rfc[(c - 1) % 2]], [Rf_])
                P.cp(hb[3], f_, [Rf_], [Rhb])
                P.ts(r1, f_, -1.0, ALU.mult, [Rf_], [R])
                P.cp(hb[0], r1, [R], [Rhb])
                P.cp(r2, hb[0], [Rhb], [R])
                P.tt(r1, r1, r2, ALU.subtract, [R], [R])
                P.cp(hb[1], r1, [R], [Rhb])
                P.cp(r2, hb[1], [Rhb], [R])
                P.tt(r1, r1, r2, ALU.subtract, [R], [R])
                P.cp(hb[2], r1, [R], [Rhb])
                for i in range(3):
                    P.dma("pool", kaug[:, i, cs_], hb[i], s_s, reads=[Rhb])
                    P.dma("pool", qaug[:, i, cs_], oneb, s_s, reads=[R])
                P.dma("pool", kaug[:, 3, cs_], oneb, s_s, reads=[R])
                P.dma("pool", qaug[:, 3, cs_], hb[3], s_s, reads=[Rhb])
            P.barrier()

        def phase_attn(l):
            KT = [BA[:, i * NT:(i + 1) * NT] for i in range(2)]
            VA = [BA[:, (2 + i) * NT:(3 + i) * NT].rearrange("p (k n) -> p k n", k=64) for i in range(2)]
            o = 4 * NT
            ptr = [BA[:, o + i * 1024:o + (i + 1) * 1024] for i in range(3)]; o += 3072
            qt = [BA[:, o + i * 512:o + (i + 1) * 512] for i in range(2)]; o += 1024
            ob = [BA[:, o + i * 512:o + (i + 1) * 512] for i in range(2)]; o += 1024
            assert o <= 53504
            ots = [FA[:, i * 512:(i + 1) * 512] for i in range(2)]
            rec = FA[:, 1024:1536]
            Rkt = [Res("kt0"), Res("kt1")]; Rva = [Res("va0"), Res("va1")]
            Rpt = [Res(f"pt{i}") for i in range(3)]; Rqt = [Res("qt0"), Res("qt1")]; Rob = [Res("ob0"), Res("ob1")]
            Rots = [Res("ots0"), Res("ots1")]; Rrec = Res("rec")
            skt = [P.dsem("kt0"), P.dsem("kt1")]; sva = [P.dsem("va0"), P.dsem("va1")]
            sqt = [P.dsem("qt0"), P.dsem("qt1")]; sob = [P.dsem("ob0"), P.dsem("ob1")]
            S = [PSA[:, 0:1024], PSA[:, 1024:2048], PSB[:, 0:1024]]
            RS = [Res("S0", True), Res("S1", True), Res("S2", True)]
            OT = [PSB[:, 1024:1536], PSB[:, 1536:2048]]
            ROT = [Res("OT0", True), Res("OT1", True)]
            P.op("dve", lambda e: e.memset(VA[0][:, :, 64:128], 1.0), writes=[Rva[0]])
            P.op("dve", lambda e: e.memset(VA[1][:, :, 0:64], 1.0), writes=[Rva[1]])
            it = [0]
            for hh in range(16):
                fox = hh >= 8
                h = hh % 8
                D = 68 if fox else 96
                sl_ = hh % 2
                par = h % 2
                kt_, Rk_, sk_ = KT[sl_], Rkt[sl_], skt[sl_]
                va_, Rv_, sv_ = VA[sl_], Rva[sl_], sva[sl_]
                vsrc = (vf if fox else vm)
                if fox:
                    P.dma("sp", kt_[0:64, :], kf[h // 2, par * 64:(par + 1) * 64, :], sk_, writes=[Rk_])
                    P.dma("sp", kt_[64:68, :], kaug[h], sk_, writes=[Rk_])
                else:
                    P.dma("sp", kt_[0:96, :], km[h], sk_, writes=[Rk_])
                vv = vsrc.rearrange("(k p) n -> p k n", p=128)
                for kq in range(4):
                    P.dma("sp", va_[:, kq * 16:(kq + 1) * 16, par * 64:(par + 1) * 64],
                          vv[:, kq * 16:(kq + 1) * 16, h * 64:(h + 1) * 64], sv_, writes=[Rv_])
                mbase = 0 if fox else 2048
                odst = ofox if fox else omla
                for T in range(NTT):
                    ts_ = slice(T * TT, (T + 1) * TT)
                    qi = it[0] % 2
                    it[0] += 1
                    q_, Rq_, sq_ = qt[qi], Rqt[qi], sqt[qi]
                    if fox:
                        P.dma("sp", q_[0:64, :], qf[h // 2, par * 64:(par + 1) * 64, ts_], sq_, writes=[Rq_])
                        P.dma("sp", q_[64:68, :], qaug[h, :, ts_], sq_, writes=[Rq_])
                    else:
                        P.dma("sp", q_[0:96, :], qm[h, :, ts_], sq_, writes=[Rq_])
                    ng = 2 * (T + 1)
                    ot_, Rot_ = OT[qi], ROT[qi]

                    def qk(g):
                        s_, Rs_ = S[g % 3], RS[g % 3]
                        for i in range(2):
                            ktile = 2 * g + i
                            diag = ktile >= 4 * T
                            P.mm(s_[:, i * 512:(i + 1) * 512], kt_[0:D, ktile * 128:(ktile + 1) * 128], q_[0:D, :],
                                 True, not diag, [Rk_, Rq_], [Rs_])
                            if diag:
                                d = ktile - 4 * T
                                P.mm(s_[:, i * 512:(i + 1) * 512], ident, msk[:, mbase + d * 512:mbase + (d + 1) * 512],
                                     False, True, [Rc], [Rs_])

                    qk(0)
                    for g in range(ng):
                        if g + 1 < ng:
                            qk(g + 1)
                        s_, Rs_ = S[g % 3], RS[g % 3]
                        p_, Rp_ = ptr[g % 3], Rpt[g % 3]
                        P.act(p_, s_, AF.Exp, [Rs_, Rc], [Rp_], bias=negm[:, 0:1])
                        for i in range(2):
                            ktile = 2 * g + i
                            P.mm(ot_, va_[:, ktile, :], p_[:, i * 512:(i + 1) * 512], ktile == 0, ktile == 2 * ng - 1,
                                 [Rv_, Rp_], [Rot_])
                    o_s, Ro_s = ots[qi], Rots[qi]
                    P.cp(o_s, ot_, [Rot_], [Ro_s])
                    gd = ng % 3
                    s_, Rs_ = S[gd], RS[gd]
                    P.mm(s_[:, 0:512], (eod_f if par else eev_f), o_s, True, True, [Ro_s, Rc], [Rs_])
                    rows = slice(par * 64, (par + 1) * 64)
                    P.op("dve", lambda e, s_=s_, rows=rows: e.reciprocal(out=rec[rows, :], in_=s_[rows, 0:512]),
                         [Rs_], [Rrec])
                    ob_, Rob_, sob_ = ob[qi], Rob[qi], sob[qi]
                    P.tt(ob_[rows, :], o_s[rows, :], rec[rows, :], ALU.mult, [Ro_s, Rrec], [Rob_])
                    P.dma("pool", odst[h // 2, rows, ts_], ob_[rows, :], sob_, reads=[Rob_])
            P.barrier()

        def phase_C(l, xsrc):
            o = 0
            Wo = BA[:, o:o + 8192].rearrange("p (c n) -> p c n", c=8); o += 8192
            f = 0
            Wr = FA[:, f:f + 288].rearrange("p (c n) -> p c n", c=8); f += 288
            identf = FA[:, f:f + 128]; f += 128
            Rw = Res("wC")
            sw = P.dsem("wC")
            off = dict(o=o, f=f)

            def alloc(pi):
                o = off["o"]; f = off["f"]
                o_t = BA[:, o:o + 4096].rearrange("p (c n) -> p c n", c=8); o += 4096
                sq = BA[:, o:o + 4096].rearrange("p (c n) -> p c n", c=8); o += 4096
                yb = BA[:, o:o + 4096].rearrange("p (c n) -> p c n", c=8); o += 4096
                hb_ = BA[:, o:o + 4096].rearrange("p (c n) -> p c n", c=8); o += 4096
                assert o <= 53504
                x_t = FA[:, f:f + 4096].rearrange("p (c n) -> p c n", c=8); f += 4096
                h2f = FA[:, f:f + 4096].rearrange("p (c n) -> p c n", c=8); f += 4096
                rs = FA[:, f:f + 512]; f += 512
                g_t = FA[0:32, f:f + 512]; f += 512
                L = FA[:, f:f + 36]; f += 36
                EL = FA[:, f:f + 32]; f += 32
                EL2 = FA[:, f:f + 32]; f += 32
                oh1 = FA[:, f:f + 32]; f += 32
                oh2 = FA[:, f:f + 32]; f += 32
                G = FA[:, f:f + 32]; f += 32
                sm = FA[:, f:f + 16]; f += 16
                eg = FA[:, f:f + 4]; f += 4
                ohg = FA[:, f:f + 4]; f += 4
                assert f <= 22528
                Ro = Res("om"); Rsq = Res("sq"); Ryb = Res("yb"); Rhb_ = Res("h2b"); Rx = Res("xt")
                Rh2f = Res("h2f"); Rrs = Res("rs"); Rg_t = Res("gts"); Rr = Res("rt")
                som_ = P.dsem(f"om{pi}"); sx_ = P.dsem(f"cxt{pi}"); sxs_ = P.dsem(f"cxs{pi}")
                sh2_ = P.dsem(f"h2b{pi}"); sg_ = P.dsem(f"gts{pi}")
                off["o"] = o; off["f"] = f
                d = dict(locals())
                for k_ in ("o", "f", "pi"):
                    d.pop(k_)
                return d

            bufs = [alloc(0), alloc(1)]
            P.dma("pool", Wo, w_out[l].rearrange("(c p) n -> p c n", p=128), sw, writes=[Rw])
            P.dma("sp", Wr, w_rt[l].rearrange("(c p) n -> p c n", p=128), P.dsem("wC_hw"), writes=[Rw])
            P.cp(identf, ident, [Rc], [Rw])
            bk = [0]

            def bank():
                i = bk[0]
                bk[0] = (i + 1) % 8
                return banks[i], bres[i]

            xv = xsrc.rearrange("(c p) t -> p c t", p=128)
            xav = xa.rearrange("(c p) t -> p c t", p=128)
            h2v = h2d.rearrange("(c p) t -> p c t", p=128)

            def tile(t, o_t, sq, yb, hb_, x_t, h2f, rs, g_t, L, EL, EL2, oh1, oh2, G, sm, eg, ohg,
                     Ro, Rsq, Ryb, Rhb_, Rx, Rh2f, Rrs, Rg_t, Rr, som_, sx_, sxs_, sh2_, sg_, **_):
                ts_ = slice(t * TT, (t + 1) * TT)
                for c in range(0, 8, 2):
                    P.dma("sp", x_t[:, c:c + 2, :], xv[:, c:c + 2, ts_], sx_, writes=[Rx])
                P.dma("sp", o_t[:, 0:4, :], omla[:, :, ts_].rearrange("c p t -> p c t"), som_, writes=[Ro])
                P.dma("sp", o_t[:, 4:8, :], ofox[:, :, ts_].rearrange("c p t -> p c t"), som_, writes=[Ro])
                for grp in range(2):
                    for c in range(4):
                        P.act(sq[:, grp * 4 + c, :], o_t[:, grp * 4 + c, :], AF.Square, [Ro], [Rsq])
                    pb, Rb = bank()
                    for c in range(4):
                        P.mm(pb, ones, sq[:, grp * 4 + c, :], c == 0, c == 3, [Rsq, Rc], [Rb])
                    rstd_from_ps(pb, 128, 512.0, rs, [Rb], [Rrs], rs)
                    for c in range(4):
                        cc = grp * 4 + c
                        P.stt(yb[:, cc, :], o_t[:, cc, :], pcol(l, "g_out", cc), rs, ALU.mult, ALU.mult,
                              [Ro, Rrs, Rc], [Ryb])
                for co in range(8):
                    pb, Rb = bank()
                    for c in range(8):
                        P.mm(pb, Wo[:, c, co * 128:(co + 1) * 128], yb[:, c, :], c == 0, c == 7, [Rw, Ryb], [Rb])
                    P.tt(x_t[:, co, :], x_t[:, co, :], pb, ALU.add, [Rx, Rb], [Rx])
                for c in range(0, 8, 2):
                    P.dma("pool", xav[:, c:c + 2, ts_], x_t[:, c:c + 2, :], sxs_, reads=[Rx])
                for c in range(8):
                    P.act(sq[:, c, :], x_t[:, c, :], AF.Square, [Rx], [Rsq])
                pb, Rb = bank()
                for c in range(8):
                    P.mm(pb, ones, sq[:, c, :], c == 0, c == 7, [Rsq, Rc], [Rb])
                rstd_from_ps(pb, 128, 1024.0, rs, [Rb], [Rrs], rs)
                for c in range(8):
                    P.stt(h2f[:, c, :], x_t[:, c, :], pcol(l, "g_ffn", c), rs, ALU.mult, ALU.mult,
                          [Rx, Rrs, Rc], [Rh2f])
                    P.cp(hb_[:, c, :], h2f[:, c, :], [Rh2f], [Rhb_], eng="pool")
                for c in range(0, 8, 2):
                    P.dma("pool", h2v[:, c:c + 2, ts_], hb_[:, c:c + 2, :], sh2_, reads=[Rhb_])
                for s in range(4):
                    pb, Rb = bank()
                    for c in range(8):
                        P.mm(pb[:, 0:36], h2f[:, c, s * 128:(s + 1) * 128], Wr[:, c, :], c == 0, c == 7, [Rh2f, Rw], [Rb])
                    P.cp(L, pb[:, 0:36], [Rb], [Rr])
                    gmax, ngmax, Z, pg, m1, m2, d_, e2, g1, g2 = [sm[:, i:i + 1] for i in range(10)]
                    P.op("dve", lambda e: e.reduce_max(out=gmax, in_=L[:, 0:4], axis=AX.X), [Rr], [Rr])
                    P.ts(ngmax, gmax, -1.0, ALU.mult, [Rr], [Rr])
                    P.act(eg, L[:, 0:4], AF.Exp, [Rr], [Rr], bias=ngmax)
                    P.op("dve", lambda e: e.reduce_sum(out=Z, in_=eg, axis=AX.X), [Rr], [Rr])
                    P.op("dve", lambda e: e.reciprocal(out=pg, in_=Z), [Rr], [Rr])
                    P.ts(ohg, L[:, 0:4], gmax, ALU.is_equal, [Rr], [Rr])
                    P.ts(ohg, ohg, BIG, ALU.mult, [Rr], [Rr], s2=BIG, op1=ALU.subtract)
                    for g in range(4):
                        P.ts(EL[:, g * 8:(g + 1) * 8], L[:, 4 + g * 8:12 + g * 8], ohg[:, g:g + 1], ALU.add, [Rr], [Rr])
                    P.op("dve", lambda e: e.reduce_max(out=m1, in_=EL, axis=AX.X), [Rr], [Rr])
                    P.ts(oh1, EL, m1, ALU.is_equal, [Rr], [Rr])
                    P.stt(EL2, oh1, -BIG, EL, ALU.mult, ALU.add, [Rr], [Rr])
                    P.op("dve", lambda e: e.reduce_max(out=m2, in_=EL2, axis=AX.X), [Rr], [Rr])
                    P.ts(oh2, EL2, m2, ALU.is_equal, [Rr], [Rr])
                    P.tt(d_, m2, m1, ALU.subtract, [Rr], [Rr])
                    P.act(e2, d_, AF.Exp, [Rr], [Rr])
                    P.ts(d_, e2, 1.0, ALU.add, [Rr], [Rr])
                    P.op("dve", lambda e: e.reciprocal(out=d_, in_=d_), [Rr], [Rr])
                    P.tt(g1, pg, d_, ALU.mult, [Rr], [Rr])
                    P.tt(g2, g1, e2, ALU.mult, [Rr], [Rr])
                    P.ts(G, oh1, g1, ALU.mult, [Rr], [Rr])
                    P.stt(G, oh2, g2, G, ALU.mult, ALU.add, [Rr], [Rr])
                    p2, R2 = bank()
                    P.mm(p2[0:32, 0:128], G, identf, True, True, [Rr, Rw], [R2])
                    P.cp(g_t[:, s * 128:(s + 1) * 128], p2[0:32, 0:128], [R2], [Rg_t])
                P.dma("pool", gTd[:, ts_], g_t, sg_, reads=[Rg_t])
            for t in range(0, NTT, 2):
                interleave(P, lambda: tile(t, **bufs[0]), lambda: tile(t + 1, **bufs[1]))
            P.barrier()

        def phase_moe(l):
            ST = 2048
            o = 0
            h2 = BA[:, o:o + 8 * ST].rearrange("p (c n) -> p c n", c=8); o += 8 * ST
            wg = [BA[:, o + i * 2048:o + (i + 1) * 2048].rearrange("p (c n) -> p c n", c=8) for i in range(2)]; o += 4096
            wu = [BA[:, o + i * 2048:o + (i + 1) * 2048].rearrange("p (c n) -> p c n", c=8) for i in range(2)]; o += 4096
            wd = [BA[:, o + i * 2048:o + (i + 1) * 2048].rearrange("p (c n) -> p c n", c=2) for i in range(2)]; o += 4096
            ab = [BA[:, o + i * 1024:o + (i + 1) * 1024].rearrange("p (c n) -> p c n", c=2) for i in range(2)]; o += 2048
            assert o <= 53504
            acc = FA[:, 0:8 * ST].rearrange("p (c n) -> p c n", c=8)
            f = 8 * ST
            gbc = [FA[:, f + i * 512:f + (i + 1) * 512] for i in range(3)]; f += 1536
            sgt = [FA[:, f + i * 512:f + (i + 1) * 512] for i in range(2)]; f += 1024
            tu = [FA[:, f + i * 512:f + (i + 1) * 512] for i in range(2)]; f += 1024
            assert f <= 22528
            Rh2 = Res("h2"); Racc = [Res(f"acc{i}") for i in range(4)]
            Rwe = [Res("we0"), Res("we1")]; Rab = [Res("ab0"), Res("ab1")]
            Rgbc = [Res(f"gbc{i}") for i in range(3)]; Rsg = [Res("sg0"), Res("sg1")]; Rtu = [Res("tu0"), Res("tu1")]
            sh = P.dsem("mh2"); sacc = P.dsem("macc"); swe = [P.dsem("we0"), P.dsem("we1")]
            sgb = [P.dsem(f"gbc{i}") for i in range(3)]; sst_ = P.dsem("mst")
            h2v = h2d.rearrange("(c p) t -> p c t", p=128)
            xav = xa.rearrange("(c p) t -> p c t", p=128)
            xbv = xb.rearrange("(c p) t -> p c t", p=128)
            PG = [banks[0], banks[1]]; RPG = [bres[0], bres[1]]
            PU = [banks[2], banks[3]]; RPU = [bres[2], bres[3]]
            PD = [banks[4], banks[5], banks[6], banks[7]]; RPD = [bres[4], bres[5], bres[6], bres[7]]
            def load_w(jb):
                st2, e2 = divmod(jb, 32)
                wi2 = jb % 2
                P.dma("pool", wg[wi2], w_eg[l, e2].rearrange("(c p) n -> p c n", p=128), swe[wi2], writes=[Rwe[wi2]])
                P.dma("pool", wu[wi2], w_eu[l, e2].rearrange("(c p) n -> p c n", p=128), swe[wi2], writes=[Rwe[wi2]])
                P.dma("pool", wd[wi2], w_ed[l, e2].rearrange("(c p) n -> p c n", p=128), swe[wi2], writes=[Rwe[wi2]])

            NJ = (NT // ST) * 32
            units = [(jb, sub) for jb in range(NJ) for sub in range(4)]

            def gu(u):
                jb, sub = units[u]
                st_, e_ = divmod(jb, 32)
                wi = jb % 2
                t0 = st_ * ST + sub * 512
                cs_ = slice(sub * 512, (sub + 1) * 512)
                k3 = u % 3
                P.dma("sp", gbc[k3], gTd[e_:e_ + 1, t0:t0 + 512].partition_broadcast(128)[:, 0, :], sgb[k3],
                      writes=[Rgbc[k3]])
                a_, Ra_ = ab[u % 2], Rab[u % 2]
                for j in range(2):
                    pg_, Rpg_ = PG[j], RPG[j]
                    pu_, Rpu_ = PU[j], RPU[j]
                    for c in range(8):
                        P.mm(pg_, wg[wi][:, c, j * 128:(j + 1) * 128], h2[:, c, cs_], c == 0, c == 7,
                             [Rwe[wi], Rh2], [Rpg_])
                    for c in range(8):
                        P.mm(pu_, wu[wi][:, c, j * 128:(j + 1) * 128], h2[:, c, cs_], c == 0, c == 7,
                             [Rwe[wi], Rh2], [Rpu_])
                    P.act(sgt[j], pg_, AF.Silu, [Rpg_], [Rsg[j]])
                    P.tt(tu[j], pu_, gbc[k3], ALU.mult, [Rpu_, Rgbc[k3]], [Rtu[j]])
                    P.tt(a_[:, j, :], sgt[j], tu[j], ALU.mult, [Rsg[j], Rtu[j]], [Ra_])

            def dn(u):
                jb, sub = units[u]
                wi = jb % 2
                cs_ = slice(sub * 512, (sub + 1) * 512)
                a_, Ra_ = ab[u % 2], Rab[u % 2]
                for co in range(8):
                    pd_, Rpd_ = PD[co % 4], RPD[co % 4]
                    for j in range(2):
                        P.mm(pd_, wd[wi][:, j, co * 128:(co + 1) * 128], a_[:, j, :], j == 0, j == 1,
                             [Rwe[wi], Ra_], [Rpd_])
                    P.tt(acc[:, co, cs_], acc[:, co, cs_], pd_, ALU.add, [Racc[sub], Rpd_], [Racc[sub]])

            def load_act(st_):
                for c in range(8):
                    P.dma("sp", h2[:, c, :], h2v[:, c, st_ * ST:(st_ + 1) * ST], sh, writes=[Rh2])
                for sub in range(4):
                    for c in range(0, 8, 4):
                        P.dma("sp", acc[:, c:c + 4, sub * 512:(sub + 1) * 512],
                              xav[:, c:c + 4, st_ * ST + sub * 512:st_ * ST + (sub + 1) * 512], sacc, writes=[Racc[sub]])

            def store_act(st_):
                for sub in range(4):
                    for c in range(0, 8, 4):
                        P.dma("pool", xbv[:, c:c + 4, st_ * ST + sub * 512:st_ * ST + (sub + 1) * 512],
                              acc[:, c:c + 4, sub * 512:(sub + 1) * 512], sst_, reads=[Racc[sub]])

            load_w(0)
            load_act(0)
            NU = len(units)
            for u in range(NU):
                jb, sub = units[u]
                st_, e_ = divmod(jb, 32)
                if sub == 0 and jb + 1 < NJ:
                    load_w(jb + 1)
                if u == 0:
                    gu(0)
                nxt_same_st = (u + 1 < NU) and (units[u + 1][0] // 32 == st_)
                if nxt_same_st:
                    gu(u + 1)
                dn(u)
                if (u + 1 < NU) and not nxt_same_st:
                    store_act(st_)
                    load_act(st_ + 1)
                    gu(u + 1)
            store_act(NT // ST - 1)
            P.barrier()

        def phase_ple(l, xdst):
            o = 0
            Wpg = BA[:, o:o + 8192].rearrange("p (c n) -> p c n", c=8); o += 8192
            Wpp = BA[:, o:o + 2048].rearrange("p (c n) -> p c n", c=2); o += 2048
            Rw = Res("wP")
            sw = P.dsem("wP")
            off = dict(o=o, f=0)

            def alloc(pi):
                o = off["o"]; f = off["f"]
                sq = BA[:, o:o + 4096].rearrange("p (c n) -> p c n", c=8); o += 4096
                h3 = BA[:, o:o + 4096].rearrange("p (c n) -> p c n", c=8); o += 4096
                pbt = BA[:, o:o + 1024].rearrange("p (c n) -> p c n", c=2); o += 1024
                assert o <= 53504
                x_t = FA[:, f:f + 4096].rearrange("p (c n) -> p c n", c=8); f += 4096
                pj = FA[:, f:f + 4096].rearrange("p (c n) -> p c n", c=8); f += 4096
                rs = FA[:, f:f + 512]; f += 512
                rp = FA[:, f:f + 512]; f += 512
                ee = [FA[:, f + i * 512:f + (i + 1) * 512] for i in range(2)]; f += 1024
                pl = [FA[:, f + i * 512:f + (i + 1) * 512] for i in range(2)]; f += 1024
                assert f <= 22528
                Rsq = Res("sq"); Rh3 = Res("h3"); Rpbt = Res("pb"); Rx = Res("xt"); Rpj = Res("pj")
                Rrs = Res("rs"); Rrp = Res("rp"); Ree = [Res("ee0"), Res("ee1")]; Rpl = [Res("pl0"), Res("pl1")]
                sx_ = P.dsem(f"pxt{pi}"); spb_ = P.dsem(f"ppb{pi}"); sxs_ = P.dsem(f"pxs{pi}")
                off["o"] = o; off["f"] = f
                d = dict(locals())
                for k_ in ("o", "f", "pi"):
                    d.pop(k_)
                return d

            bufs = [alloc(0), alloc(1)]
            P.dma("pool", Wpg, w_pg[l].rearrange("(c p) n -> p c n", p=128), sw, writes=[Rw])
            P.dma("pool", Wpp, w_pp[l].rearrange("(c p) n -> p c n", p=128), sw, writes=[Rw])
            bk = [0]

            def bank():
                i = bk[0]
                bk[0] = (i + 1) % 8
                return banks[i], bres[i]

            xv = xb.rearrange("(c p) t -> p c t", p=128)
            xdv = xdst.rearrange("(c p) t -> p c t", p=128)
            pv = pT[l].rearrange("(c p) t -> p c t", p=128)

            def tile(t, sq, h3, pbt, x_t, pj, rs, rp, ee, pl, Rsq, Rh3, Rpbt, Rx, Rpj, Rrs, Rrp, Ree, Rpl,
                     sx_, spb_, sxs_, **_):
                ts_ = slice(t * TT, (t + 1) * TT)
                for c in range(0, 8, 2):
                    P.dma("sp", x_t[:, c:c + 2, :], xv[:, c:c + 2, ts_], sx_, writes=[Rx])
                P.dma("pool", pbt, pv[:, :, ts_], spb_, writes=[Rpbt])
                for c in range(8):
                    P.act(sq[:, c, :], x_t[:, c, :], AF.Square, [Rx], [Rsq])
                pb, Rb = bank()
                for c in range(8):
                    P.mm(pb, ones, sq[:, c, :], c == 0, c == 7, [Rsq, Rc], [Rb])
                rstd_from_ps(pb, 128, 1024.0, rs, [Rb], [Rrs], rs)
                for c in range(8):
                    P.stt(h3[:, c, :], x_t[:, c, :], pcol(l, "g_plen", c), rs, ALU.mult, ALU.mult,
                          [Rx, Rrs, Rc], [Rh3])
                for co in range(8):
                    pb, Rb = bank()
                    for c in range(2):
                        P.mm(pb, Wpp[:, c, co * 128:(co + 1) * 128], pbt[:, c, :], c == 0, c == 1, [Rw, Rpbt], [Rb])
                    P.cp(pj[:, co, :], pb, [Rb], [Rpj])
                    P.act(sq[:, co, :], pb, AF.Square, [Rb], [Rsq])
                pb, Rb = bank()
                for c in range(8):
                    P.mm(pb, ones, sq[:, c, :], c == 0, c == 7, [Rsq, Rc], [Rb])
                rstd_from_ps(pb, 128, 1024.0, rp, [Rb], [Rrp], rp)
                for co in range(8):
                    pb, Rb = bank()
                    for c in range(8):
                        P.mm(pb, Wpg[:, c, co * 128:(co + 1) * 128], h3[:, c, :], c == 0, c == 7, [Rw, Rh3], [Rb])
                    e_, Re_ = ee[co % 2], Ree[co % 2]
                    p_, Rp_ = pl[co % 2], Rpl[co % 2]
                    P.act(e_, pb, AF.Exp, [Rb], [Re_], scale=-1.0)
                    P.ts(e_, e_, 1.0, ALU.add, [Re_], [Re_])
                    P.op("dve", lambda e, e_=e_: e.reciprocal(out=e_, in_=e_), [Re_], [Re_])
                    P.stt(p_, pj[:, co, :], pcol(l, "g_pleo", co), rp, ALU.mult, ALU.mult, [Rpj, Rrp, Rc], [Rp_])
                    P.tt(p_, p_, e_, ALU.mult, [Rp_, Re_], [Rp_])
                    P.tt(x_t[:, co, :], x_t[:, co, :], p_, ALU.add, [Rx, Rp_], [Rx])
                for c in range(0, 8, 2):
                    P.dma("pool", xdv[:, c:c + 2, ts_], x_t[:, c:c + 2, :], sxs_, reads=[Rx])
            for t in range(0, NTT, 2):
                interleave(P, lambda: tile(t, **bufs[0]), lambda: tile(t + 1, **bufs[1]))
            P.barrier()

        phase_rope()
        if stop == "R":
            P.emit()
            return nc
        for l in range(2):
            xsrc = xT if l == 0 else xc
            xdst = xc if l == 0 else yT
            phase_A(l, xsrc)
            if stop == f"A{l}":
                break
            phase_scan(l)
            if stop == f"S{l}":
                break
            phase_attn(l)
            if stop == f"T{l}":
                break
            phase_C(l, xsrc)
            if stop == f"C{l}":
                break
            phase_moe(l)
            if stop == f"M{l}":
                break
            phase_ple(l, xdst)
            if stop == f"P{l}":
                break
        P.barrier()
        P.emit()
    return nc


def _consts():
    c = np.zeros((128, NCST), np.float32)
    c[:, 0:128] = np.eye(128, dtype=np.float32)
    c[:, 128:256] = 1.0
    c[0:64, 256:320] = 1.0
    c[64:128, 320:384] = 1.0
    for m in range(64, 80):
        c[m + 16, 384 + m] = -1.0
    for m in range(80, 96):
        c[m - 16, 384 + m] = 1.0
    c[64, 512:576] = 1.0
    c[0, 640 + 64:640 + 128] = 1.0
    pidx = np.arange(128)[:, None]
    col = np.arange(512)[None, :]
    for d in range(4):
        kk = d * 128 + pidx
        c[:, 768 + d * 512:768 + (d + 1) * 512] = np.where(kk > col, NEG, 0.0)
        c[:, 768 + 2048 + d * 512:768 + 2048 + (d + 1) * 512] = np.where((kk // 64) * 64 > col, NEG, 0.0)
    inv = (10000.0 ** (-np.arange(0, 32, 2, dtype=np.float32) / 32)).astype(np.float32)
    c[64:80, 768 + 4096] = inv
    c[80:96, 768 + 4096] = inv
    return c


def _prm(inp):
    prm = np.zeros((2, 128, NP), np.float32)
    for l in range(2):
        def cols(v, n):
            return v.reshape(n, 128).T
        prm[l, :, 0:8] = cols(inp["g_attn_norm"][l], 8)
        prm[l, :, 8:16] = cols(inp["g_ffn_norm"][l], 8)
        prm[l, :, 16:24] = cols(inp["g_ple_norm"][l], 8)
        prm[l, :, 24:32] = cols(inp["g_ple_out"][l], 8)
        prm[l, :, 32:36] = cols(inp["g_out_mla"][l], 4)
        prm[l, :, 36:40] = cols(inp["g_out_fox"][l], 4)
        prm[l, :, 40:42] = cols(inp["g_q_lora"][l], 2)
        prm[l, :, 42] = inp["g_kv_lora"][l]
        prm[l, 0:96, 43] = inp["g_mla_q"][l]
        prm[l, 0:96, 44] = inp["g_mla_k"][l]
        prm[l, 0:64, 45] = inp["g_fox_q"][l]
        prm[l, 64:128, 45] = inp["g_fox_q"][l]
        prm[l, 0:64, 46] = inp["g_fox_k"][l]
        prm[l, 64:128, 46] = inp["g_fox_k"][l]
        prm[l, 0:8, 47] = inp["b_fox_f"][l]
    return prm


def make_in_maps(inp):
    f = lambda a: np.ascontiguousarray(a, dtype=np.float32)
    NE_ = 1 if os.environ.get("KSMALL") else 32
    shared = dict(
        prm=_prm(inp), cst=_consts(),
        w_in=f(inp["w_in"]), w_uq=f(inp["w_uq"]), w_ukv=f(inp["w_ukv"]), w_out=f(inp["w_out"]),
        w_rt=f(np.concatenate([inp["w_router_group"], inp["w_router_expert"]], axis=-1)),
        w_eg=f(inp["w_exp_gate"][:, :NE_]), w_eu=f(inp["w_exp_up"][:, :NE_]), w_ed=f(inp["w_exp_down"][:, :NE_]),
        w_pg=f(inp["w_ple_gate"]), w_pp=f(inp["w_ple_proj"]),
    )
    maps = []
    for c in range(8):
        b = c % 4
        m = dict(shared)
        m["xT"] = f(inp["x"][b].T)
        m["pT"] = f(np.transpose(inp["p"][:, b], (0, 2, 1)))
        m["pos"] = np.ascontiguousarray(inp["positions"][b][None, :], dtype=np.int32)
        maps.append(m)
    return maps


def kernel(**inputs):
    inp = {k: np.asarray(v) for k, v in inputs.items()}
    nc = build_program()
    maps = make_in_maps(inp)
    res = run_bass_kernel_spmd(nc, maps, core_ids=list(range(8)))
    out = np.stack([np.ascontiguousarray(res.results[b]["yT"].T) for b in range(4)], axis=0)
    return out.astype(np.float32)
```

```python
import contextlib
import os
import numpy as np
import concourse.bass as bass
import concourse.mybir as mybir
from concourse.bass_utils import run_bass_kernel_spmd

F32 = mybir.dt.float32
BF16 = mybir.dt.bfloat16
I32 = mybir.dt.int32
ALU = mybir.AluOpType
AF = mybir.ActivationFunctionType
AX = mybir.AxisListType

NT = 8192
TT = 512
NTT = NT // TT
EPS = 1e-6
NEG = -30000.0
M0 = 12.0
BIG = 1.0e9

COMPUTE = ("pe", "act", "dve", "pool")
ALLENG = ("pe", "act", "dve", "pool", "sp")


class Res:
    __slots__ = ("name", "w", "r", "psum")

    def __init__(self, name="", psum=False):
        self.name = name
        self.w = None
        self.r = []
        self.psum = psum


class Prog:
    def __init__(self, nc, stack):
        self.nc = nc
        self.stack = stack
        self.ops = {e: [] for e in ALLENG}
        self.cnt = {}
        self.sem = {}
        self.seen = {e: {} for e in ALLENG}
        self.hook = None
        for e in COMPUTE:
            self._mk_sem(e)

    def _mk_sem(self, key):
        self.sem[key] = self.stack.enter_context(self.nc.semaphore("s_" + str(key)))
        self.cnt[key] = 0

    def dsem(self, name):
        key = "d_" + name
        if key not in self.sem:
            self._mk_sem(key)
        return key

    def sb(self, name, shape, dt):
        return self.stack.enter_context(self.nc.sbuf_tensor(name, list(shape), dt))

    def ps(self, name, shape, dt=F32):
        return self.stack.enter_context(self.nc.psum_tensor(name, list(shape), dt))

    def _deps(self, eng, reads, writes):
        deps = []
        for r in reads:
            if r.w is not None:
                deps.append(r.w)
            if r.psum:
                deps.extend(ev for ev in r.r if ev[2] != eng)
        for w in writes:
            if w.w is not None:
                if not (eng == "pe" and w.w[2] == "pe"):
                    deps.append(w.w)
            deps.extend(w.r)
        waits = {}
        for (k, v, e) in deps:
            if self.seen[eng].get(k, 0) >= v:
                continue
            if waits.get(k, 0) < v:
                waits[k] = v
        for k, v in waits.items():
            self.seen[eng][k] = v
        return list(waits.items())

    def _commit(self, ev, reads, writes):
        for r in reads:
            r.r.append(ev)
        for w in writes:
            w.w = ev
            w.r = []

    def op(self, eng, fn, reads=(), writes=()):
        waits = self._deps(eng, reads, writes)
        self.cnt[eng] += 1
        ev = (eng, self.cnt[eng], eng)
        self.ops[eng].append((waits, fn, (eng, 1)))
        self._commit(ev, reads, writes)
        if self.hook:
            self.hook()

    def dma(self, q, out, in_, sem, reads=(), writes=(), slow=False):
        waits = self._deps(q, reads, writes)
        self.cnt[sem] += 16
        ev = (sem, self.cnt[sem], "dma")
        if slow:
            self.ops[q].append((waits, lambda e: e.dma_start(out=out, in_=in_, allow_slow_non_contiguous=True), (sem, 16)))
        else:
            self.ops[q].append((waits, lambda e: e.dma_start(out=out, in_=in_), (sem, 16)))
        self._commit(ev, reads, writes)
        if self.hook:
            self.hook()

    def barrier(self):
        for e in ALLENG:
            waits = []
            for k, v in self.cnt.items():
                if v > self.seen[e].get(k, 0):
                    waits.append((k, v))
                    self.seen[e][k] = v
            self.ops[e].append((waits, None, None))

    def mm(self, out, lhsT, rhs, start, stop, reads, writes):
        self.op("pe", lambda e: e.matmul(out, lhsT=lhsT, rhs=rhs, start=start, stop=stop), reads, writes)

    def act(self, out, in_, func, reads, writes, bias=None, scale=1.0):
        if bias is None:
            self.op("act", lambda e: e.activation(out=out, in_=in_, func=func, scale=scale), reads, writes)
        else:
            self.op("act", lambda e: e.activation(out=out, in_=in_, func=func, bias=bias, scale=scale), reads, writes)

    def stt(self, out, in0, scalar, in1, op0, op1, reads, writes, eng="dve"):
        self.op(eng, lambda e: e.scalar_tensor_tensor(out=out, in0=in0, scalar=scalar, in1=in1, op0=op0, op1=op1),
                reads, writes)

    def tt(self, out, in0, in1, op, reads, writes, eng="dve"):
        self.op(eng, lambda e: e.tensor_tensor(out=out, in0=in0, in1=in1, op=op), reads, writes)

    def ts(self, out, in0, s1, op0, reads, writes, s2=None, op1=None, eng="dve"):
        if op1 is None:
            self.op(eng, lambda e: e.tensor_single_scalar(out=out, in_=in0, scalar=s1, op=op0), reads, writes)
        else:
            self.op(eng, lambda e: e.tensor_scalar(out=out, in0=in0, scalar1=s1, scalar2=s2, op0=op0, op1=op1),
                    reads, writes)

    def cp(self, out, in_, reads, writes, eng="dve"):
        self.op(eng, lambda e: e.tensor_copy(out=out, in_=in_), reads, writes)

    def emit(self):
        nc = self.nc
        handles = {"pe": "tensor", "act": "scalar", "dve": "vector", "pool": "gpsimd", "sp": "sync"}
        with nc.Block() as block:
            for en in ALLENG:
                ops = self.ops[en]

                def body(eng, ops=ops):
                    for waits, fn, inc in ops:
                        for k, v in waits:
                            eng.wait_ge(self.sem[k], v)
                        if fn is None:
                            continue
                        fn(eng).then_inc(self.sem[inc[0]], inc[1])
                getattr(block, handles[en])(body)


def interleave(P, f0, f1, every=4):
    import threading
    sems = [threading.Semaphore(0), threading.Semaphore(0)]
    done = [False, False]
    cnt = [0]
    cur = [0]
    err = []

    def hook():
        cnt[0] += 1
        if cnt[0] >= every:
            cnt[0] = 0
            me = cur[0]
            other = 1 - me
            if not done[other]:
                cur[0] = other
                sems[other].release()
                sems[me].acquire()

    def run(i, f):
        sems[i].acquire()
        try:
            f()
        except BaseException as ex:
            err.append(ex)
        finally:
            done[i] = True
            other = 1 - i
            if not done[other]:
                cur[0] = other
                sems[other].release()

    th = [threading.Thread(target=run, args=(i, f)) for i, f in enumerate((f0, f1))]
    P.hook = hook
    for t in th:
        t.start()
    sems[0].release()
    for t in th:
        t.join()
    P.hook = None
    if err:
        raise err[0]


class Ring:
    def __init__(self, P, name, shape, dt, n, psum=False):
        self.t = []
        self.r = []
        for i in range(n):
            nm = f"{name}{i}"
            self.t.append(P.ps(nm, shape, dt) if psum else P.sb(nm, shape, dt))
            self.r.append(Res(nm))
        self.sems = [P.dsem(f"{name}{i}") for i in range(n)] if not psum else None
        self.i = 0
        self.n = n

    def next(self):
        i = self.i
        self.i = (i + 1) % self.n
        if self.sems:
            return self.t[i], self.r[i], self.sems[i]
        return self.t[i], self.r[i]


PC = dict(g_attn=0, g_ffn=8, g_plen=16, g_pleo=24, g_out=32, g_ql=40, g_kvl=42, g_mq=43, g_mk=44,
          g_fq=45, g_fk=46, b_f=47)
NP = 48
CC = dict(ident=0, ones=128, bd64=256, rot=384, eeven=512, eodd=640, mfr=768, mch=768 + 2048, freq=768 + 4096)
NCST = 768 + 4096 + 1


def build_program(dbg=None):
    nc = bass.Bass("TRN2", target_bir_lowering=False)
    stop = (dbg or {}).get("stop")
    douts = (dbg or {}).get("outs", [])

    def din(name, shape, dt=F32):
        return nc.dram_tensor(name, list(shape), dt, kind="ExternalInput").ap()

    def dint(name, shape, dt):
        kind = "ExternalOutput" if name in douts else "Internal"
        return nc.dram_tensor(name, list(shape), dt, kind=kind).ap()

    xT = din("xT", [1024, NT])
    pT = din("pT", [2, 256, NT])
    pos = din("pos", [1, NT], I32)
    prm_d = din("prm", [2, 128, NP])
    cst_d = din("cst", [128, NCST])
    w_in = din("w_in", [2, 1024, 1960])
    w_uq = din("w_uq", [2, 256, 768])
    w_ukv = din("w_ukv", [2, 128, 1024])
    w_out = din("w_out", [2, 1024, 1024])
    w_rt = din("w_rt", [2, 1024, 36])
    NE_ = 1 if os.environ.get("KSMALL") else 32
    w_eg = din("w_eg", [2, NE_, 1024, 256])
    w_eu = din("w_eu", [2, NE_, 1024, 256])
    w_ed = din("w_ed", [2, NE_, 256, 1024])
    w_pg = din("w_pg", [2, 1024, 1024])
    w_pp = din("w_pp", [2, 256, 1024])
    yT = nc.dram_tensor("yT", [1024, NT], F32, kind="ExternalOutput").ap()

    xa = dint("xa", [1024, NT], F32)
    xb = dint("xb", [1024, NT], F32)
    xc = dint("xc", [1024, NT], F32)
    ropeT = dint("ropeT", [2, 96, NT], F32)
    qm = dint("qm", [8, 96, NT], BF16)
    km = dint("km", [8, 96, NT], BF16)
    vm = dint("vm", [NT, 512], BF16)
    qf = dint("qf", [4, 128, NT], BF16)
    kf = dint("kf", [4, 128, NT], BF16)
    vf = dint("vf", [NT, 512], BF16)
    lfd = dint("lfd", [8, NT], F32)
    kaug = dint("kaug", [8, 4, NT], BF16)
    qaug = dint("qaug", [8, 4, NT], BF16)
    omla = dint("omla", [4, 128, NT], BF16)
    ofox = dint("ofox", [4, 128, NT], BF16)
    h2d = dint("h2d", [1024, NT], BF16)
    gTd = dint("gTd", [32, NT], F32)

    with contextlib.ExitStack() as st:
        P = Prog(nc, st)
        BA = P.sb("BA", [128, 53504], BF16)
        FA = P.sb("FA", [128, 22528], F32)
        PSA = P.ps("PSA", [128, 2048], F32)
        PSB = P.ps("PSB", [128, 2048], F32)
        banks = [PSA[:, i * 512:(i + 1) * 512] for i in range(4)] + [PSB[:, i * 512:(i + 1) * 512] for i in range(4)]
        bres = [Res(f"bank{i}", psum=True) for i in range(8)]
        cst = P.sb("cst_sb", [128, 768], BF16)
        cstf = P.sb("cstf", [128, 257], F32)
        msk = P.sb("msk", [128, 4096], BF16)
        prm = P.sb("prm_sb", [128, 2 * NP], F32)
        negm = P.sb("negm", [128, 1], F32)
        kc = P.sb("kc", [128, 2], F32)
        Rc = Res("const")
        sem_c = P.dsem("const")
        sem_cp = P.dsem("const_swdge")
        P.dma("pool", cst[:, 0:768], cst_d[:, 0:768], sem_cp, writes=[Rc])
        P.dma("pool", msk[:, :], cst_d[:, 768:768 + 4096], sem_cp, writes=[Rc])
        P.dma("sp", cstf[:, 0:256], cst_d[:, 512:768], sem_c, writes=[Rc])
        P.dma("sp", cstf[:, 256:257], cst_d[:, 768 + 4096:768 + 4097], sem_c, writes=[Rc], slow=True)
        P.dma("sp", prm[:, 0:NP], prm_d[0], sem_c, writes=[Rc])
        P.dma("sp", prm[:, NP:2 * NP], prm_d[1], sem_c, writes=[Rc])
        P.op("dve", lambda e: e.memset(negm[:], -M0), writes=[Rc])
        P.op("dve", lambda e: e.memset(kc[:, 0:1], EPS), writes=[Rc])
        P.op("dve", lambda e: e.memset(kc[:, 1:2], 1.0), writes=[Rc])
        ident = cst[:, 0:128]
        ones = cst[:, 128:256]
        bd64 = cst[:, 256:384]
        rot = cst[0:96, 384:480]
        eev_f = cstf[:, 0:128]
        eod_f = cstf[:, 128:256]
        freq = cstf[0:96, 256:257]

        def pcol(l, name, c=0, rows=slice(0, 128)):
            j = l * NP + PC[name] + c
            return prm[rows, j:j + 1]

        for l in range(2):
            P.ts(pcol(l, "g_mq"), pcol(l, "g_mq"), float(96 ** -0.5), ALU.mult, [Rc], [Rc])
            P.ts(pcol(l, "g_fq"), pcol(l, "g_fq"), float(64 ** -0.5), ALU.mult, [Rc], [Rc])
        P.barrier()

        def rstd_from_ps(psb, rows, n, out, rd, wr, tmp):
            P.act(tmp, psb, AF.Ln, list(rd) + [Rc], wr, bias=kc[0:rows, 0:1], scale=1.0 / n)
            P.act(out, tmp, AF.Exp, wr, wr, scale=-0.5)

        def phase_rope():
            posi = FA[0:96, 0:512].bitcast(I32)
            posf = FA[0:96, 512:1024]
            ang = FA[0:96, 1024:1536]
            cs = FA[0:96, 1536:2048]
            sn = FA[0:96, 2048:2560]
            ki = FA[0:96, 2560:3072].bitcast(I32)
            kf = FA[0:96, 3072:3584]
            R = Res("rope")
            sl = P.dsem("rope_l")
            ss_ = P.dsem("rope_s")
            for t in range(NTT):
                ts_ = slice(t * TT, (t + 1) * TT)
                P.dma("sp", posi, pos[0:1, ts_].partition_broadcast(96), sl, writes=[R])
                P.cp(posf, posi, [R], [R])
                P.ts(ang, posf, freq, ALU.mult, [R, Rc], [R])
                for (dst, off) in ((cs, 0.25), (sn, 0.0)):
                    P.ts(posf, ang, float(1.0 / (2 * np.pi)), ALU.mult, [R], [R], s2=float(off), op1=ALU.add)
                    P.cp(ki, posf, [R], [R])
                    P.cp(kf, ki, [R], [R])
                    P.tt(posf, posf, kf, ALU.subtract, [R], [R])
                    P.ts(kf, posf, 0.5, ALU.is_gt, [R], [R])
                    P.tt(posf, posf, kf, ALU.subtract, [R], [R])
                    P.act(dst, posf, AF.Sin, [R], [R], scale=float(2 * np.pi))
                P.dma("pool", ropeT[0, 64:96, ts_], cs[64:96, :], ss_, reads=[R])
                P.dma("pool", ropeT[1, 64:96, ts_], sn[64:96, :], ss_, reads=[R])
            P.barrier()

        def phase_A(l, xsrc):
            o = 0
            Win = BA[:, o:o + 8 * 1960].rearrange("p (c n) -> p c n", c=8); o += 8 * 1960
            Wkpe = BA[:, o:o + 8 * 96].rearrange("p (c n) -> p c n", c=8); o += 8 * 96
            Wuq = BA[:, o:o + 2 * 768].rearrange("p (c n) -> p c n", c=2); o += 2 * 768
            Wukv = BA[:, o:o + 1024]; o += 1024
            Wv = BA[:, o:o + 512].rearrange("p (h d) -> p h d", h=8); o += 512
            stg = [BA[:, o + i * 512:o + (i + 1) * 512] for i in range(6)]; o += 3072
            Rw = Res("wA"); Rstg = [Res(f"stg{i}") for i in range(6)]
            sw = P.dsem("wA")
            sst = [P.dsem(f"stg{i}") for i in range(6)]
            off = dict(o=o, f=0)

            def alloc(pi):
                o = off["o"]; f = off["f"]
                sq = BA[:, o:o + 8 * 512].rearrange("p (c n) -> p c n", c=8); o += 4096
                ht = BA[:, o:o + 8 * 512].rearrange("p (c n) -> p c n", c=8); o += 4096
                qcn = BA[:, o:o + 1024].rearrange("p (c n) -> p c n", c=2); o += 1024
                kvn = BA[:, o:o + 512]; o += 512
                sqs = [BA[:, o + i * 512:o + (i + 1) * 512] for i in range(2)]; o += 1024
                kpg = BA[:, o:o + 512]; o += 512
                vst = [BA[:, o + i * 2048:o + (i + 1) * 2048].rearrange("p (s n) -> p s n", s=4) for i in range(2)]; o += 4096
                assert o <= 53504
                x_t = FA[:, f:f + 4096].rearrange("p (c n) -> p c n", c=8); f += 4096
                rstd = FA[:, f:f + 512]; f += 512
                tmp = FA[:, f:f + 512]; f += 512
                qcr = FA[:, f:f + 1024].rearrange("p (c n) -> p c n", c=2); f += 1024
                rr = [FA[:, f + i * 512:f + (i + 1) * 512] for i in range(2)]; f += 1024
                t1 = FA[:, f:f + 512]; f += 512
                t2 = FA[:, f:f + 512]; f += 512
                kr = FA[:, f:f + 512]; f += 512
                cs = FA[:, f:f + 512]; f += 512
                sn = FA[:, f:f + 512]; f += 512
                lz = FA[:, f:f + 512]; f += 512
                la = FA[:, f:f + 512]; f += 512
                assert f <= 22528
                Rsq = Res("sq"); Rht = Res("ht"); Rqcn = Res("qcn"); Rkvn = Res("kvn")
                Rsqs = [Res("sqs0"), Res("sqs1")]; Rkpg = Res("kpg")
                Rvst = [Res("vst0"), Res("vst1")]; Rx = Res("xt"); Rrstd = Res("rstd"); Rtmp = Res("tmp")
                Rqcr = Res("qcr"); Rrr = [Res("rr0"), Res("rr1")]; Rt1 = Res("t1"); Rt2 = Res("t2"); Rkr = Res("kr")
                Rcs = Res("cs"); Rl = Res("lz")
                sxx = P.dsem(f"xt{pi}")
                scs = P.dsem(f"cs{pi}")
                svs = [P.dsem(f"vst0_{pi}"), P.dsem(f"vst1_{pi}")]
                sl = P.dsem(f"lz{pi}")
                P.op("dve", lambda e: e.memset(kpg, 0.0), writes=[Rkpg])
                off["o"] = o; off["f"] = f
                d = dict(locals())
                for k_ in ("o", "f", "pi"):
                    d.pop(k_)
                return d

            bufs = [alloc(0), alloc(1)]
            wv = w_in[l].rearrange("(c p) n -> p c n", p=128)
            for c in range(8):
                P.dma("pool", Win[:, c, :], wv[:, c, :], sw, writes=[Rw])
            P.dma("pool", Wuq, w_uq[l].rearrange("(c p) n -> p c n", p=128), sw, writes=[Rw])
            P.dma("pool", Wukv, w_ukv[l], sw, writes=[Rw])
            P.dma("pool", Wv, w_ukv[l].rearrange("p (h two d) -> p h two d", h=8, two=2)[:, :, 1, :], sw, writes=[Rw])
            P.op("dve", lambda e: e.memset(Wkpe, 0.0), writes=[Rw])
            P.cp(Wkpe[:, :, 64:96], Win[:, :, 384:416], [Rw], [Rw])
            stg_i = [0]

            def stage():
                i = stg_i[0]
                stg_i[0] = (i + 1) % 6
                return stg[i], Rstg[i], sst[i]

            bk = [0]

            def bank():
                i = bk[0]
                bk[0] = (i + 1) % 8
                return banks[i], bres[i]

            xv = xsrc.rearrange("(c p) t -> p c t", p=128)
            KS = int(os.environ.get('KA_STOP', '99'))

            def tile(t, sq, ht, qcn, kvn, sqs, kpg, vst, x_t, rstd, tmp, qcr, rr, t1, t2, kr, cs, sn, lz, la,
                     Rsq, Rht, Rqcn, Rkvn, Rsqs, Rkpg, Rvst, Rx, Rrstd, Rtmp, Rqcr, Rrr, Rt1, Rt2, Rkr, Rcs, Rl,
                     sxx, scs, svs, sl, **_):
                ts_ = slice(t * TT, (t + 1) * TT)
                for c in range(0, 8, 2):
                    P.dma("sp", x_t[:, c:c + 2, :], xv[:, c:c + 2, ts_], sxx, writes=[Rx])
                P.dma("sp", cs[64:96, :], ropeT[0, 64:96, ts_], scs, writes=[Rcs])
                P.dma("sp", sn[64:96, :], ropeT[1, 64:96, ts_], scs, writes=[Rcs])
                for c in range(8):
                    P.act(sq[:, c, :], x_t[:, c, :], AF.Square, [Rx], [Rsq])
                pb, Rb = bank()
                for c in range(8):
                    P.mm(pb, ones, sq[:, c, :], c == 0, c == 7, [Rsq, Rc], [Rb])
                rstd_from_ps(pb, 128, 1024.0, rstd, [Rb], [Rrstd], rstd)
                for c in range(8):
                    P.stt(ht[:, c, :], x_t[:, c, :], pcol(l, "g_attn", c), rstd, ALU.mult, ALU.mult,
                          [Rx, Rrstd, Rc], [Rht])
                if KS <= 1:
                    return
                pq = []
                for j in range(2):
                    pb, Rb = bank()
                    for c in range(8):
                        P.mm(pb, Win[:, c, j * 128:(j + 1) * 128], ht[:, c, :], c == 0, c == 7, [Rw, Rht], [Rb])
                    P.cp(qcr[:, j, :], pb, [Rb], [Rqcr])
                    P.act(sq[:, j, :], pb, AF.Square, [Rb], [Rsq])
                pb, Rb = bank()
                for j in range(2):
                    P.mm(pb, ones, sq[:, j, :], j == 0, j == 1, [Rsq, Rc], [Rb])
                rstd_from_ps(pb, 128, 256.0, tmp, [Rb], [Rtmp], tmp)
                for j in range(2):
                    P.stt(qcn[:, j, :], qcr[:, j, :], pcol(l, "g_ql", j), tmp, ALU.mult, ALU.mult,
                          [Rqcr, Rtmp, Rc], [Rqcn])
                if KS <= 2:
                    return
                pkv, Rkv = bank()
                for c in range(8):
                    P.mm(pkv, Win[:, c, 256:384], ht[:, c, :], c == 0, c == 7, [Rw, Rht], [Rkv])
                P.act(sq[:, 2, :], pkv, AF.Square, [Rkv], [Rsq])
                pb, Rb = bank()
                P.mm(pb, ones, sq[:, 2, :], True, True, [Rsq, Rc], [Rb])
                rstd_from_ps(pb, 128, 128.0, tmp, [Rb], [Rtmp], tmp)
                P.stt(kvn, pkv, pcol(l, "g_kvl"), tmp, ALU.mult, ALU.mult, [Rkv, Rtmp, Rc], [Rkvn])
                if KS <= 3:
                    return
                for h in range(8):
                    pb, Rb = bank()
                    for j in range(2):
                        P.mm(pb[0:96, :], Wuq[:, j, h * 96:(h + 1) * 96], qcn[:, j, :], j == 0, j == 1, [Rw, Rqcn], [Rb])
                    s_, Rs_ = sqs[h % 2], Rsqs[h % 2]
                    P.act(s_[0:96, :], pb[0:96, :], AF.Square, [Rb], [Rs_])
                    p2, R2 = bank()
                    P.mm(p2[0:96, :], ones[0:96, 0:96], s_[0:96, :], True, True, [Rs_, Rc], [R2])
                    r_, Rr_ = rr[h % 2], Rrr[h % 2]
                    rstd_from_ps(p2[0:96, :], 96, 96.0, r_[0:96, :], [R2], [Rr_], r_[0:96, :])
                    so, Rso, sso = stage()
                    P.stt(so[0:96, :], pb[0:96, :], pcol(l, "g_mq", 0, slice(0, 96)), r_[0:96, :], ALU.mult, ALU.mult,
                          [Rb, Rr_, Rc], [Rso])
                    p3, R3 = bank()
                    P.mm(p3[0:96, :], rot, so[0:96, :], True, True, [Rso, Rc], [R3])
                    P.tt(t1[64:96, :], so[64:96, :], cs[64:96, :], ALU.mult, [Rso, Rcs], [Rt1])
                    P.tt(t2[64:96, :], p3[64:96, :], sn[64:96, :], ALU.mult, [R3, Rcs], [Rt2])
                    P.tt(so[64:96, :], t1[64:96, :], t2[64:96, :], ALU.add, [Rt1, Rt2], [Rso])
                    P.dma("pool", qm[h, :, ts_], so[0:96, :], sso, reads=[Rso])
                if KS <= 4:
                    return
                pkp, Rkp = bank()
                for c in range(8):
                    P.mm(pkp[0:96, :], Wkpe[:, c, :], ht[:, c, :], c == 0, c == 7, [Rw, Rht], [Rkp])
                for i in range(2):
                    P.act(sqs[i][64:96, :], pkp[64:96, :], AF.Square, [Rkp], [Rsqs[i]])
                P.ts(kpg[64:96, :], pkp[64:96, :], pcol(l, "g_mk", 0, slice(64, 96)), ALU.mult, [Rkp, Rc], [Rkpg])
                p3, R3 = bank()
                P.mm(p3[0:96, :], rot, kpg[0:96, :], True, True, [Rkpg, Rc], [R3])
                P.tt(t1[64:96, :], kpg[64:96, :], cs[64:96, :], ALU.mult, [Rkpg, Rcs], [Rt1])
                P.tt(t2[64:96, :], p3[64:96, :], sn[64:96, :], ALU.mult, [R3, Rcs], [Rt2])
                P.tt(kr[64:96, :], t1[64:96, :], t2[64:96, :], ALU.add, [Rt1, Rt2], [Rkr])
                for h in range(8):
                    pb, Rb = bank()
                    P.mm(pb[0:64, :], Wukv[:, h * 128:h * 128 + 64], kvn, True, True, [Rw, Rkvn], [Rb])
                    s_, Rs_ = sqs[h % 2], Rsqs[h % 2]
                    P.act(s_[0:64, :], pb[0:64, :], AF.Square, [Rb], [Rs_])
                    p2, R2 = bank()
                    P.mm(p2[0:96, :], ones[0:96, 0:96], s_[0:96, :], True, True, [Rs_, Rc], [R2])
                    r_, Rr_ = rr[h % 2], Rrr[h % 2]
                    rstd_from_ps(p2[0:96, :], 96, 96.0, r_[0:96, :], [R2], [Rr_], r_[0:96, :])
                    so, Rso, sso = stage()
                    P.stt(so[0:64, :], pb[0:64, :], pcol(l, "g_mk", 0, slice(0, 64)), r_[0:64, :], ALU.mult, ALU.mult,
                          [Rb, Rr_, Rc], [Rso])
                    P.tt(so[64:96, :], kr[64:96, :], r_[64:96, :], ALU.mult, [Rkr, Rr_], [Rso])
                    P.dma("pool", km[h, :, ts_], so[0:96, :], sso, reads=[Rso])
                if KS <= 5:
                    return
                v_, Rv_, sv_ = vst[0], Rvst[0], svs[0]
                for s in range(4):
                    pb, Rb = bank()
                    P.mm(pb, kvn[:, s * 128:(s + 1) * 128], Wv.rearrange("p h d -> p (h d)"), True, True, [Rw, Rkvn], [Rb])
                    P.cp(v_[:, s, :], pb, [Rb], [Rv_])
                P.dma("pool", vm[ts_, :].rearrange("(s p) n -> p s n", p=128), v_, sv_, reads=[Rv_])
                if KS <= 6:
                    return
                for (dst, col0, gname) in ((qf, 416, "g_fq"), (kf, 928, "g_fk")):
                    for j in range(4):
                        pb, Rb = bank()
                        for c in range(8):
                            P.mm(pb, Win[:, c, col0 + j * 128:col0 + (j + 1) * 128], ht[:, c, :], c == 0, c == 7,
                                 [Rw, Rht], [Rb])
                        s_, Rs_ = sqs[j % 2], Rsqs[j % 2]
                        P.act(s_, pb, AF.Square, [Rb], [Rs_])
                        p2, R2 = bank()
                        P.mm(p2, bd64, s_, True, True, [Rs_, Rc], [R2])
                        r_, Rr_ = rr[j % 2], Rrr[j % 2]
                        rstd_from_ps(p2, 128, 64.0, r_, [R2], [Rr_], r_)
                        so, Rso, sso = stage()
                        P.stt(so, pb, pcol(l, gname), r_, ALU.mult, ALU.mult, [Rb, Rr_, Rc], [Rso])
                        P.dma("pool", dst[j, :, ts_], so, sso, reads=[Rso])
                if KS <= 7:
                    return
                v_, Rv_, sv_ = vst[1], Rvst[1], svs[1]
                for s in range(4):
                    pb, Rb = bank()
                    for c in range(8):
                        P.mm(pb, ht[:, c, s * 128:(s + 1) * 128], Win[:, c, 1440:1952], c == 0, c == 7, [Rw, Rht], [Rb])
                    P.cp(v_[:, s, :], pb, [Rb], [Rv_])
                P.dma("pool", vf[ts_, :].rearrange("(s p) n -> p s n", p=128), v_, sv_, reads=[Rv_])
                if KS <= 8:
                    return
                pb, Rb = bank()
                for c in range(8):
                    P.mm(pb[0:8, :], Win[:, c, 1952:1960], ht[:, c, :], c == 0, c == 7, [Rw, Rht], [Rb])
                P.ts(lz[0:8, :], pb[0:8, :], pcol(l, "b_f", 0, slice(0, 8)), ALU.add, [Rb, Rc], [Rl])
                P.ts(la[0:8, :], lz[0:8, :], -1.0, ALU.mult, [Rl], [Rl])
                P.tt(la[0:8, :], la[0:8, :], lz[0:8, :], ALU.max, [Rl], [Rl])
                P.act(la[0:8, :], la[0:8, :], AF.Exp, [Rl], [Rl], scale=-1.0)
                P.act(la[0:8, :], la[0:8, :], AF.Ln, [Rl, Rc], [Rl], bias=kc[0:8, 1:2])
                P.ts(lz[0:8, :], lz[0:8, :], 0.0, ALU.min, [Rl], [Rl])
                P.tt(lz[0:8, :], lz[0:8, :], la[0:8, :], ALU.subtract, [Rl], [Rl])
                P.dma("pool", lfd[:, ts_], lz[0:8, :], sl, reads=[Rl])
            for t in range(0, int(os.environ.get('KA_TILES', NTT)), 2):
                interleave(P, lambda: tile(t, **bufs[0]), lambda: tile(t + 1, **bufs[1]))
            P.barrier()

        def phase_scan(l):
            CH = 2048
            lf = [FA[0:8, i * CH:(i + 1) * CH] for i in range(2)]
            fc = [FA[0:8, (2 + i) * CH:(3 + i) * CH] for i in range(2)]
            r1 = FA[0:8, 4 * CH:5 * CH]
            r2 = FA[0:8, 5 * CH:6 * CH]
            onesf = FA[0:8, 6 * CH:7 * CH]
            hb = [BA[0:8, i * CH:(i + 1) * CH] for i in range(4)]
            oneb = BA[0:8, 4 * CH:5 * CH]
            R = Res("scan"); Rlf = [Res("lf0"), Res("lf1")]; Rfc = [Res("fc0"), Res("fc1")]
            Rhb = Res("hb")
            s_l = [P.dsem("lf0"), P.dsem("lf1")]
            s_s = P.dsem("scan_s")
            P.op("dve", lambda e: e.memset(onesf, 1.0), writes=[R])
            P.op("dve", lambda e: e.memset(oneb, 1.0), writes=[R])
            for c in range(NT // CH):
                cs_ = slice(c * CH, (c + 1) * CH)
                l_, Rl_ = lf[c % 2], Rlf[c % 2]
                f_, Rf_ = fc[c % 2], Rfc[c % 2]
                P.dma("sp", l_, lfd[:, cs_], s_l[c % 2], writes=[Rl_])
                if c == 0:
                    P.op("dve", lambda e, f_=f_, l_=l_: e.tensor_tensor_scan(out=f_, data0=onesf, data1=l_, initial=0.0,
                                                                             op0=ALU.mult, op1=ALU.add),
                         [R, Rl_], [Rf_])
                else:
                    pf = fc[(c - 1) % 2]
                    P.op("dve", lambda e, f_=f_, l_=l_, pf=pf: e.tensor_tensor_scan(
                        out=f_, data0=onesf, data1=l_, initial=pf[:, CH - 1:CH], op0=ALU.mult, op1=ALU.add),
                        [R, Rl_, Rfc[(c - 1) % 2]], [Rf_])
                P.cp(hb[3], f_, [Rf_], [Rhb])
                P.ts(r1, f_, -1.0, ALU.mult, [Rf_], [R])
                P.cp(hb[0], r1, [R], [Rhb])
                P.cp(r2, hb[0], [Rhb], [R])
                P.tt(r1, r1, r2, ALU.subtract, [R], [R])
                P.cp(hb[1], r1, [R], [Rhb])
                P.cp(r2, hb[1], [Rhb], [R])
                P.tt(r1, r1, r2, ALU.subtract, [R], [R])
                P.cp(hb[2], r1, [R], [Rhb])
                for i in range(3):
                    P.dma("pool", kaug[:, i, cs_], hb[i], s_s, reads=[Rhb])
                    P.dma("pool", qaug[:, i, cs_], oneb, s_s, reads=[R])
                P.dma("pool", kaug[:, 3, cs_], oneb, s_s, reads=[R])
                P.dma("pool", qaug[:, 3, cs_], hb[3], s_s, reads=[Rhb])
            P.barrier()

        def phase_attn(l):
            KT = [BA[:, i * NT:(i + 1) * NT] for i in range(2)]
            VA = [BA[:, (2 + i) * NT:(3 + i) * NT].rearrange("p (k n) -> p k n", k=64) for i in range(2)]
            o = 4 * NT
            ptr = [BA[:, o + i * 1024:o + (i + 1) * 1024] for i in range(3)]; o += 3072
            qt = [BA[:, o + i * 512:o + (i + 1) * 512] for i in range(2)]; o += 1024
            ob = [BA[:, o + i * 512:o + (i + 1) * 512] for i in range(2)]; o += 1024
            assert o <= 53504
            ots = [FA[:, i * 512:(i + 1) * 512] for i in range(2)]
            rec = FA[:, 1024:1536]
            Rkt = [Res("kt0"), Res("kt1")]; Rva = [Res("va0"), Res("va1")]
            Rpt = [Res(f"pt{i}") for i in range(3)]; Rqt = [Res("qt0"), Res("qt1")]; Rob = [Res("ob0"), Res("ob1")]
            Rots = [Res("ots0"), Res("ots1")]; Rrec = Res("rec")
            skt = [P.dsem("kt0"), P.dsem("kt1")]; sva = [P.dsem("va0"), P.dsem("va1")]
            sqt = [P.dsem("qt0"), P.dsem("qt1")]; sob = [P.dsem("ob0"), P.dsem("ob1")]
            S = [PSA[:, 0:1024], PSA[:, 1024:2048], PSB[:, 0:1024]]
            RS = [Res("S0", True), Res("S1", True), Res("S2", True)]
            OT = [PSB[:, 1024:1536], PSB[:, 1536:2048]]
            ROT = [Res("OT0", True), Res("OT1", True)]
            P.op("dve", lambda e: e.memset(VA[0][:, :, 64:128], 1.0), writes=[Rva[0]])
            P.op("dve", lambda e: e.memset(VA[1][:, :, 0:64], 1.0), writes=[Rva[1]])
            it = [0]
            for hh in range(16):
                fox = hh >= 8
                h = hh % 8
                D = 68 if fox else 96
                sl_ = hh % 2
                par = h % 2
                kt_, Rk_, sk_ = KT[sl_], Rkt[sl_], skt[sl_]
                va_, Rv_, sv_ = VA[sl_], Rva[sl_], sva[sl_]
                vsrc = (vf if fox else vm)
                if fox:
                    P.dma("sp", kt_[0:64, :], kf[h // 2, par * 64:(par + 1) * 64, :], sk_, writes=[Rk_])
                    P.dma("sp", kt_[64:68, :], kaug[h], sk_, writes=[Rk_])
                else:
                    P.dma("sp", kt_[0:96, :], km[h], sk_, writes=[Rk_])
                vv = vsrc.rearrange("(k p) n -> p k n", p=128)
                for kq in range(4):
                    P.dma("sp", va_[:, kq * 16:(kq + 1) * 16, par * 64:(par + 1) * 64],
                          vv[:, kq * 16:(kq + 1) * 16, h * 64:(h + 1) * 64], sv_, writes=[Rv_])
                mbase = 0 if fox else 2048
                odst = ofox if fox else omla
                for T in range(NTT):
                    ts_ = slice(T * TT, (T + 1) * TT)
                    qi = it[0] % 2
                    it[0] += 1
                    q_, Rq_, sq_ = qt[qi], Rqt[qi], sqt[qi]
                    if fox:
                        P.dma("sp", q_[0:64, :], qf[h // 2, par * 64:(par + 1) * 64, ts_], sq_, writes=[Rq_])
                        P.dma("sp", q_[64:68, :], qaug[h, :, ts_], sq_, writes=[Rq_])
                    else:
                        P.dma("sp", q_[0:96, :], qm[h, :, ts_], sq_, writes=[Rq_])
                    ng = 2 * (T + 1)
                    ot_, Rot_ = OT[qi], ROT[qi]

                    def qk(g):
                        s_, Rs_ = S[g % 3], RS[g % 3]
                        for i in range(2):
                            ktile = 2 * g + i
                            diag = ktile >= 4 * T
                            P.mm(s_[:, i * 512:(i + 1) * 512], kt_[0:D, ktile * 128:(ktile + 1) * 128], q_[0:D, :],
                                 True, not diag, [Rk_, Rq_], [Rs_])
                            if diag:
                                d = ktile - 4 * T
                                P.mm(s_[:, i * 512:(i + 1) * 512], ident, msk[:, mbase + d * 512:mbase + (d + 1) * 512],
                                     False, True, [Rc], [Rs_])

                    qk(0)
                    qk(1)
                    for g in range(ng):
                        if g + 2 < ng:
                            qk(g + 2)
                        s_, Rs_ = S[g % 3], RS[g % 3]
                        p_, Rp_ = ptr[g % 3], Rpt[g % 3]
                        P.act(p_, s_, AF.Exp, [Rs_, Rc], [Rp_], bias=negm[:, 0:1])
                        for i in range(2):
                            ktile = 2 * g + i
                            P.mm(ot_, va_[:, ktile, :], p_[:, i * 512:(i + 1) * 512], ktile == 0, ktile == 2 * ng - 1,
                                 [Rv_, Rp_], [Rot_])
                    o_s, Ro_s = ots[qi], Rots[qi]
                    P.cp(o_s, ot_, [Rot_], [Ro_s])
                    gd = ng % 3
                    s_, Rs_ = S[gd], RS[gd]
                    P.mm(s_[:, 0:512], (eod_f if par else eev_f), o_s, True, True, [Ro_s, Rc], [Rs_])
                    rows = slice(par * 64, (par + 1) * 64)
                    P.op("dve", lambda e, s_=s_, rows=rows: e.reciprocal(out=rec[rows, :], in_=s_[rows, 0:512]),
                         [Rs_], [Rrec])
                    ob_, Rob_, sob_ = ob[qi], Rob[qi], sob[qi]
                    P.tt(ob_[rows, :], o_s[rows, :], rec[rows, :], ALU.mult, [Ro_s, Rrec], [Rob_])
                    P.dma("pool", odst[h // 2, rows, ts_], ob_[rows, :], sob_, reads=[Rob_])
            P.barrier()

        def phase_C(l, xsrc):
            o = 0
            Wo = BA[:, o:o + 8192].rearrange("p (c n) -> p c n", c=8); o += 8192
            f = 0
            Wr = FA[:, f:f + 288].rearrange("p (c n) -> p c n", c=8); f += 288
            identf = FA[:, f:f + 128]; f += 128
            Rw = Res("wC")
            sw = P.dsem("wC")
            off = dict(o=o, f=f)

            def alloc(pi):
                o = off["o"]; f = off["f"]
                o_t = BA[:, o:o + 4096].rearrange("p (c n) -> p c n", c=8); o += 4096
                sq = BA[:, o:o + 4096].rearrange("p (c n) -> p c n", c=8); o += 4096
                yb = BA[:, o:o + 4096].rearrange("p (c n) -> p c n", c=8); o += 4096
                hb_ = BA[:, o:o + 4096].rearrange("p (c n) -> p c n", c=8); o += 4096
                assert o <= 53504
                x_t = FA[:, f:f + 4096].rearrange("p (c n) -> p c n", c=8); f += 4096
                h2f = FA[:, f:f + 4096].rearrange("p (c n) -> p c n", c=8); f += 4096
                rs = FA[:, f:f + 512]; f += 512
                g_t = FA[0:32, f:f + 512]; f += 512
                L = FA[:, f:f + 36]; f += 36
                EL = FA[:, f:f + 32]; f += 32
                EL2 = FA[:, f:f + 32]; f += 32
                oh1 = FA[:, f:f + 32]; f += 32
                oh2 = FA[:, f:f + 32]; f += 32
                G = FA[:, f:f + 32]; f += 32
                sm = FA[:, f:f + 16]; f += 16
                eg = FA[:, f:f + 4]; f += 4
                ohg = FA[:, f:f + 4]; f += 4
                assert f <= 22528
                Ro = Res("om"); Rsq = Res("sq"); Ryb = Res("yb"); Rhb_ = Res("h2b"); Rx = Res("xt")
                Rh2f = Res("h2f"); Rrs = Res("rs"); Rg_t = Res("gts"); Rr = Res("rt")
                som_ = P.dsem(f"om{pi}"); sx_ = P.dsem(f"cxt{pi}"); sxs_ = P.dsem(f"cxs{pi}")
                sh2_ = P.dsem(f"h2b{pi}"); sg_ = P.dsem(f"gts{pi}")
                off["o"] = o; off["f"] = f
                d = dict(locals())
                for k_ in ("o", "f", "pi"):
                    d.pop(k_)
                return d

            bufs = [alloc(0), alloc(1)]
            P.dma("pool", Wo, w_out[l].rearrange("(c p) n -> p c n", p=128), sw, writes=[Rw])
            P.dma("sp", Wr, w_rt[l].rearrange("(c p) n -> p c n", p=128), P.dsem("wC_hw"), writes=[Rw])
            P.cp(identf, ident, [Rc], [Rw])
            bk = [0]

            def bank():
                i = bk[0]
                bk[0] = (i + 1) % 8
                return banks[i], bres[i]

            xv = xsrc.rearrange("(c p) t -> p c t", p=128)
            xav = xa.rearrange("(c p) t -> p c t", p=128)
            h2v = h2d.rearrange("(c p) t -> p c t", p=128)

            def tile(t, o_t, sq, yb, hb_, x_t, h2f, rs, g_t, L, EL, EL2, oh1, oh2, G, sm, eg, ohg,
                     Ro, Rsq, Ryb, Rhb_, Rx, Rh2f, Rrs, Rg_t, Rr, som_, sx_, sxs_, sh2_, sg_, **_):
                ts_ = slice(t * TT, (t + 1) * TT)
                for c in range(0, 8, 2):
                    P.dma("sp", x_t[:, c:c + 2, :], xv[:, c:c + 2, ts_], sx_, writes=[Rx])
                P.dma("sp", o_t[:, 0:4, :], omla[:, :, ts_].rearrange("c p t -> p c t"), som_, writes=[Ro])
                P.dma("sp", o_t[:, 4:8, :], ofox[:, :, ts_].rearrange("c p t -> p c t"), som_, writes=[Ro])
                for grp in range(2):
                    for c in range(4):
                        P.act(sq[:, grp * 4 + c, :], o_t[:, grp * 4 + c, :], AF.Square, [Ro], [Rsq])
                    pb, Rb = bank()
                    for c in range(4):
                        P.mm(pb, ones, sq[:, grp * 4 + c, :], c == 0, c == 3, [Rsq, Rc], [Rb])
                    rstd_from_ps(pb, 128, 512.0, rs, [Rb], [Rrs], rs)
                    for c in range(4):
                        cc = grp * 4 + c
                        P.stt(yb[:, cc, :], o_t[:, cc, :], pcol(l, "g_out", cc), rs, ALU.mult, ALU.mult,
                              [Ro, Rrs, Rc], [Ryb])
                for co in range(8):
                    pb, Rb = bank()
                    for c in range(8):
                        P.mm(pb, Wo[:, c, co * 128:(co + 1) * 128], yb[:, c, :], c == 0, c == 7, [Rw, Ryb], [Rb])
                    P.tt(x_t[:, co, :], x_t[:, co, :], pb, ALU.add, [Rx, Rb], [Rx])
                for c in range(0, 8, 2):
                    P.dma("pool", xav[:, c:c + 2, ts_], x_t[:, c:c + 2, :], sxs_, reads=[Rx])
                for c in range(8):
                    P.act(sq[:, c, :], x_t[:, c, :], AF.Square, [Rx], [Rsq])
                pb, Rb = bank()
                for c in range(8):
                    P.mm(pb, ones, sq[:, c, :], c == 0, c == 7, [Rsq, Rc], [Rb])
                rstd_from_ps(pb, 128, 1024.0, rs, [Rb], [Rrs], rs)
                for c in range(8):
                    P.stt(h2f[:, c, :], x_t[:, c, :], pcol(l, "g_ffn", c), rs, ALU.mult, ALU.mult,
                          [Rx, Rrs, Rc], [Rh2f])
                    P.cp(hb_[:, c, :], h2f[:, c, :], [Rh2f], [Rhb_], eng="pool")
                for c in range(0, 8, 2):
                    P.dma("pool", h2v[:, c:c + 2, ts_], hb_[:, c:c + 2, :], sh2_, reads=[Rhb_])
                for s in range(4):
                    pb, Rb = bank()
                    for c in range(8):
                        P.mm(pb[:, 0:36], h2f[:, c, s * 128:(s + 1) * 128], Wr[:, c, :], c == 0, c == 7, [Rh2f, Rw], [Rb])
                    P.cp(L, pb[:, 0:36], [Rb], [Rr])
                    gmax, ngmax, Z, pg, m1, m2, d_, e2, g1, g2 = [sm[:, i:i + 1] for i in range(10)]
                    P.op("dve", lambda e: e.reduce_max(out=gmax, in_=L[:, 0:4], axis=AX.X), [Rr], [Rr])
                    P.ts(ngmax, gmax, -1.0, ALU.mult, [Rr], [Rr])
                    P.act(eg, L[:, 0:4], AF.Exp, [Rr], [Rr], bias=ngmax)
                    P.op("dve", lambda e: e.reduce_sum(out=Z, in_=eg, axis=AX.X), [Rr], [Rr])
                    P.op("dve", lambda e: e.reciprocal(out=pg, in_=Z), [Rr], [Rr])
                    P.ts(ohg, L[:, 0:4], gmax, ALU.is_equal, [Rr], [Rr])
                    P.ts(ohg, ohg, BIG, ALU.mult, [Rr], [Rr], s2=BIG, op1=ALU.subtract)
                    for g in range(4):
                        P.ts(EL[:, g * 8:(g + 1) * 8], L[:, 4 + g * 8:12 + g * 8], ohg[:, g:g + 1], ALU.add, [Rr], [Rr])
                    P.op("dve", lambda e: e.reduce_max(out=m1, in_=EL, axis=AX.X), [Rr], [Rr])
                    P.ts(oh1, EL, m1, ALU.is_equal, [Rr], [Rr])
                    P.stt(EL2, oh1, -BIG, EL, ALU.mult, ALU.add, [Rr], [Rr])
                    P.op("dve", lambda e: e.reduce_max(out=m2, in_=EL2, axis=AX.X), [Rr], [Rr])
                    P.ts(oh2, EL2, m2, ALU.is_equal, [Rr], [Rr])
                    P.tt(d_, m2, m1, ALU.subtract, [Rr], [Rr])
                    P.act(e2, d_, AF.Exp, [Rr], [Rr])
                    P.ts(d_, e2, 1.0, ALU.add, [Rr], [Rr])
                    P.op("dve", lambda e: e.reciprocal(out=d_, in_=d_), [Rr], [Rr])
                    P.tt(g1, pg, d_, ALU.mult, [Rr], [Rr])
                    P.tt(g2, g1, e2, ALU.mult, [Rr], [Rr])
                    P.ts(G, oh1, g1, ALU.mult, [Rr], [Rr])
                    P.stt(G, oh2, g2, G, ALU.mult, ALU.add, [Rr], [Rr])
                    p2, R2 = bank()
                    P.mm(p2[0:32, 0:128], G, identf, True, True, [Rr, Rw], [R2])
                    P.cp(g_t[:, s * 128:(s + 1) * 128], p2[0:32, 0:128], [R2], [Rg_t])
                P.dma("pool", gTd[:, ts_], g_t, sg_, reads=[Rg_t])
            for t in range(0, NTT, 2):
                interleave(P, lambda: tile(t, **bufs[0]), lambda: tile(t + 1, **bufs[1]))
            P.barrier()

        def phase_moe(l):
            ST = 2048
            o = 0
            h2 = BA[:, o:o + 8 * ST].rearrange("p (c n) -> p c n", c=8); o += 8 * ST
            wg = [BA[:, o + i * 2048:o + (i + 1) * 2048].rearrange("p (c n) -> p c n", c=8) for i in range(2)]; o += 4096
            wu = [BA[:, o + i * 2048:o + (i + 1) * 2048].rearrange("p (c n) -> p c n", c=8) for i in range(2)]; o += 4096
            wd = [BA[:, o + i * 2048:o + (i + 1) * 2048].rearrange("p (c n) -> p c n", c=2) for i in range(2)]; o += 4096
            ab = [BA[:, o + i * 1024:o + (i + 1) * 1024].rearrange("p (c n) -> p c n", c=2) for i in range(2)]; o += 2048
            assert o <= 53504
            acc = FA[:, 0:8 * ST].rearrange("p (c n) -> p c n", c=8)
            f = 8 * ST
            gbc = [FA[:, f + i * 512:f + (i + 1) * 512] for i in range(3)]; f += 1536
            sgt = [FA[:, f + i * 512:f + (i + 1) * 512] for i in range(2)]; f += 1024
            tu = [FA[:, f + i * 512:f + (i + 1) * 512] for i in range(2)]; f += 1024
            assert f <= 22528
            Rh2 = Res("h2"); Racc = [Res(f"acc{i}") for i in range(4)]
            Rwe = [Res("we0"), Res("we1")]; Rab = [Res("ab0"), Res("ab1")]
            Rgbc = [Res(f"gbc{i}") for i in range(3)]; Rsg = [Res("sg0"), Res("sg1")]; Rtu = [Res("tu0"), Res("tu1")]
            sh = P.dsem("mh2"); sacc = P.dsem("macc"); swe = [P.dsem("we0"), P.dsem("we1")]
            sgb = [P.dsem(f"gbc{i}") for i in range(3)]; sst_ = P.dsem("mst")
            h2v = h2d.rearrange("(c p) t -> p c t", p=128)
            xav = xa.rearrange("(c p) t -> p c t", p=128)
            xbv = xb.rearrange("(c p) t -> p c t", p=128)
            PG = [banks[0], banks[1]]; RPG = [bres[0], bres[1]]
            PU = [banks[2], banks[3]]; RPU = [bres[2], bres[3]]
            PD = [banks[4], banks[5], banks[6], banks[7]]; RPD = [bres[4], bres[5], bres[6], bres[7]]
            def load_w(jb):
                st2, e2 = divmod(jb, 32)
                wi2 = jb % 2
                P.dma("pool", wg[wi2], w_eg[l, e2].rearrange("(c p) n -> p c n", p=128), swe[wi2], writes=[Rwe[wi2]])
                P.dma("pool", wu[wi2], w_eu[l, e2].rearrange("(c p) n -> p c n", p=128), swe[wi2], writes=[Rwe[wi2]])
                P.dma("pool", wd[wi2], w_ed[l, e2].rearrange("(c p) n -> p c n", p=128), swe[wi2], writes=[Rwe[wi2]])

            NJ = (NT // ST) * 32
            units = [(jb, sub) for jb in range(NJ) for sub in range(4)]

            def gu(u):
                jb, sub = units[u]
                st_, e_ = divmod(jb, 32)
                wi = jb % 2
                t0 = st_ * ST + sub * 512
                cs_ = slice(sub * 512, (sub + 1) * 512)
                k3 = u % 3
                P.dma("sp", gbc[k3], gTd[e_:e_ + 1, t0:t0 + 512].partition_broadcast(128)[:, 0, :], sgb[k3],
                      writes=[Rgbc[k3]])
                a_, Ra_ = ab[u % 2], Rab[u % 2]
                for j in range(2):
                    pg_, Rpg_ = PG[j], RPG[j]
                    pu_, Rpu_ = PU[j], RPU[j]
                    for c in range(8):
                        P.mm(pg_, wg[wi][:, c, j * 128:(j + 1) * 128], h2[:, c, cs_], c == 0, c == 7,
                             [Rwe[wi], Rh2], [Rpg_])
                    for c in range(8):
                        P.mm(pu_, wu[wi][:, c, j * 128:(j + 1) * 128], h2[:, c, cs_], c == 0, c == 7,
                             [Rwe[wi], Rh2], [Rpu_])
                    P.act(sgt[j], pg_, AF.Silu, [Rpg_], [Rsg[j]])
                    P.tt(tu[j], pu_, gbc[k3], ALU.mult, [Rpu_, Rgbc[k3]], [Rtu[j]])
                    P.tt(a_[:, j, :], sgt[j], tu[j], ALU.mult, [Rsg[j], Rtu[j]], [Ra_])

            def dn(u):
                jb, sub = units[u]
                wi = jb % 2
                cs_ = slice(sub * 512, (sub + 1) * 512)
                a_, Ra_ = ab[u % 2], Rab[u % 2]
                for co in range(8):
                    pd_, Rpd_ = PD[co % 4], RPD[co % 4]
                    for j in range(2):
                        P.mm(pd_, wd[wi][:, j, co * 128:(co + 1) * 128], a_[:, j, :], j == 0, j == 1,
                             [Rwe[wi], Ra_], [Rpd_])
                    P.tt(acc[:, co, cs_], acc[:, co, cs_], pd_, ALU.add, [Racc[sub], Rpd_], [Racc[sub]])

            def load_act(st_):
                for c in range(8):
                    P.dma("sp", h2[:, c, :], h2v[:, c, st_ * ST:(st_ + 1) * ST], sh, writes=[Rh2])
                for sub in range(4):
                    for c in range(0, 8, 4):
                        P.dma("sp", acc[:, c:c + 4, sub * 512:(sub + 1) * 512],
                              xav[:, c:c + 4, st_ * ST + sub * 512:st_ * ST + (sub + 1) * 512], sacc, writes=[Racc[sub]])

            def store_act(st_):
                for sub in range(4):
                    for c in range(0, 8, 4):
                        P.dma("pool", xbv[:, c:c + 4, st_ * ST + sub * 512:st_ * ST + (sub + 1) * 512],
                              acc[:, c:c + 4, sub * 512:(sub + 1) * 512], sst_, reads=[Racc[sub]])

            load_w(0)
            load_act(0)
            NU = len(units)
            for u in range(NU):
                jb, sub = units[u]
                st_, e_ = divmod(jb, 32)
                if sub == 0 and jb + 1 < NJ:
                    load_w(jb + 1)
                if u == 0:
                    gu(0)
                nxt_same_st = (u + 1 < NU) and (units[u + 1][0] // 32 == st_)
                if nxt_same_st:
                    gu(u + 1)
                dn(u)
                if (u + 1 < NU) and not nxt_same_st:
                    store_act(st_)
                    load_act(st_ + 1)
                    gu(u + 1)
            store_act(NT // ST - 1)
            P.barrier()

        def phase_ple(l, xdst):
            o = 0
            Wpg = BA[:, o:o + 8192].rearrange("p (c n) -> p c n", c=8); o += 8192
            Wpp = BA[:, o:o + 2048].rearrange("p (c n) -> p c n", c=2); o += 2048
            Rw = Res("wP")
            sw = P.dsem("wP")
            off = dict(o=o, f=0)

            def alloc(pi):
                o = off["o"]; f = off["f"]
                sq = BA[:, o:o + 4096].rearrange("p (c n) -> p c n", c=8); o += 4096
                h3 = BA[:, o:o + 4096].rearrange("p (c n) -> p c n", c=8); o += 4096
                pbt = BA[:, o:o + 1024].rearrange("p (c n) -> p c n", c=2); o += 1024
                assert o <= 53504
                x_t = FA[:, f:f + 4096].rearrange("p (c n) -> p c n", c=8); f += 4096
                pj = FA[:, f:f + 4096].rearrange("p (c n) -> p c n", c=8); f += 4096
                rs = FA[:, f:f + 512]; f += 512
                rp = FA[:, f:f + 512]; f += 512
                ee = [FA[:, f + i * 512:f + (i + 1) * 512] for i in range(2)]; f += 1024
                pl = [FA[:, f + i * 512:f + (i + 1) * 512] for i in range(2)]; f += 1024
                assert f <= 22528
                Rsq = Res("sq"); Rh3 = Res("h3"); Rpbt = Res("pb"); Rx = Res("xt"); Rpj = Res("pj")
                Rrs = Res("rs"); Rrp = Res("rp"); Ree = [Res("ee0"), Res("ee1")]; Rpl = [Res("pl0"), Res("pl1")]
                sx_ = P.dsem(f"pxt{pi}"); spb_ = P.dsem(f"ppb{pi}"); sxs_ = P.dsem(f"pxs{pi}")
                off["o"] = o; off["f"] = f
                d = dict(locals())
                for k_ in ("o", "f", "pi"):
                    d.pop(k_)
                return d

            bufs = [alloc(0), alloc(1)]
            P.dma("pool", Wpg, w_pg[l].rearrange("(c p) n -> p c n", p=128), sw, writes=[Rw])
            P.dma("pool", Wpp, w_pp[l].rearrange("(c p) n -> p c n", p=128), sw, writes=[Rw])
            bk = [0]

            def bank():
                i = bk[0]
                bk[0] = (i + 1) % 8
                return banks[i], bres[i]

            xv = xb.rearrange("(c p) t -> p c t", p=128)
            xdv = xdst.rearrange("(c p) t -> p c t", p=128)
            pv = pT[l].rearrange("(c p) t -> p c t", p=128)

            def tile(t, sq, h3, pbt, x_t, pj, rs, rp, ee, pl, Rsq, Rh3, Rpbt, Rx, Rpj, Rrs, Rrp, Ree, Rpl,
                     sx_, spb_, sxs_, **_):
                ts_ = slice(t * TT, (t + 1) * TT)
                for c in range(0, 8, 2):
                    P.dma("sp", x_t[:, c:c + 2, :], xv[:, c:c + 2, ts_], sx_, writes=[Rx])
                P.dma("pool", pbt, pv[:, :, ts_], spb_, writes=[Rpbt])
                for c in range(8):
                    P.act(sq[:, c, :], x_t[:, c, :], AF.Square, [Rx], [Rsq])
                pb, Rb = bank()
                for c in range(8):
                    P.mm(pb, ones, sq[:, c, :], c == 0, c == 7, [Rsq, Rc], [Rb])
                rstd_from_ps(pb, 128, 1024.0, rs, [Rb], [Rrs], rs)
                for c in range(8):
                    P.stt(h3[:, c, :], x_t[:, c, :], pcol(l, "g_plen", c), rs, ALU.mult, ALU.mult,
                          [Rx, Rrs, Rc], [Rh3])
                for co in range(8):
                    pb, Rb = bank()
                    for c in range(2):
                        P.mm(pb, Wpp[:, c, co * 128:(co + 1) * 128], pbt[:, c, :], c == 0, c == 1, [Rw, Rpbt], [Rb])
                    P.cp(pj[:, co, :], pb, [Rb], [Rpj])
                    P.act(sq[:, co, :], pb, AF.Square, [Rb], [Rsq])
                pb, Rb = bank()
                for c in range(8):
                    P.mm(pb, ones, sq[:, c, :], c == 0, c == 7, [Rsq, Rc], [Rb])
                rstd_from_ps(pb, 128, 1024.0, rp, [Rb], [Rrp], rp)
                for co in range(8):
                    pb, Rb = bank()
                    for c in range(8):
                        P.mm(pb, Wpg[:, c, co * 128:(co + 1) * 128], h3[:, c, :], c == 0, c == 7, [Rw, Rh3], [Rb])
                    e_, Re_ = ee[co % 2], Ree[co % 2]
                    p_, Rp_ = pl[co % 2], Rpl[co % 2]
                    P.act(e_, pb, AF.Exp, [Rb], [Re_], scale=-1.0)
                    P.ts(e_, e_, 1.0, ALU.add, [Re_], [Re_])
                    P.op("dve", lambda e, e_=e_: e.reciprocal(out=e_, in_=e_), [Re_], [Re_])
                    P.stt(p_, pj[:, co, :], pcol(l, "g_pleo", co), rp, ALU.mult, ALU.mult, [Rpj, Rrp, Rc], [Rp_])
                    P.tt(p_, p_, e_, ALU.mult, [Rp_, Re_], [Rp_])
                    P.tt(x_t[:, co, :], x_t[:, co, :], p_, ALU.add, [Rx, Rp_], [Rx])
                for c in range(0, 8, 2):
                    P.dma("pool", xdv[:, c:c + 2, ts_], x_t[:, c:c + 2, :], sxs_, reads=[Rx])
            for t in range(0, NTT, 2):
                interleave(P, lambda: tile(t, **bufs[0]), lambda: tile(t + 1, **bufs[1]))
            P.barrier()

        phase_rope()
        if stop == "R":
            P.emit()
            return nc
        for l in range(2):
            xsrc = xT if l == 0 else xc
            xdst = xc if l == 0 else yT
            phase_A(l, xsrc)
            if stop == f"A{l}":
                break
            phase_scan(l)
            if stop == f"S{l}":
                break
            phase_attn(l)
            if stop == f"T{l}":
                break
            phase_C(l, xsrc)
            if stop == f"C{l}":
                break
            phase_moe(l)
            if stop == f"M{l}":
                break
            phase_ple(l, xdst)
            if stop == f"P{l}":
                break
        P.barrier()
        P.emit()
    return nc


def _consts():
    c = np.zeros((128, NCST), np.float32)
    c[:, 0:128] = np.eye(128, dtype=np.float32)
    c[:, 128:256] = 1.0
    c[0:64, 256:320] = 1.0
    c[64:128, 320:384] = 1.0
    for m in range(64, 80):
        c[m + 16, 384 + m] = -1.0
    for m in range(80, 96):
        c[m - 16, 384 + m] = 1.0
    c[64, 512:576] = 1.0
    c[0, 640 + 64:640 + 128] = 1.0
    pidx = np.arange(128)[:, None]
    col = np.arange(512)[None, :]
    for d in range(4):
        kk = d * 128 + pidx
        c[:, 768 + d * 512:768 + (d + 1) * 512] = np.where(kk > col, NEG, 0.0)
        c[:, 768 + 2048 + d * 512:768 + 2048 + (d + 1) * 512] = np.where((kk // 64) * 64 > col, NEG, 0.0)
    inv = (10000.0 ** (-np.arange(0, 32, 2, dtype=np.float32) / 32)).astype(np.float32)
    c[64:80, 768 + 4096] = inv
    c[80:96, 768 + 4096] = inv
    return c


def _prm(inp):
    prm = np.zeros((2, 128, NP), np.float32)
    for l in range(2):
        def cols(v, n):
            return v.reshape(n, 128).T
        prm[l, :, 0:8] = cols(inp["g_attn_norm"][l], 8)
        prm[l, :, 8:16] = cols(inp["g_ffn_norm"][l], 8)
        prm[l, :, 16:24] = cols(inp["g_ple_norm"][l], 8)
        prm[l, :, 24:32] = cols(inp["g_ple_out"][l], 8)
        prm[l, :, 32:36] = cols(inp["g_out_mla"][l], 4)
        prm[l, :, 36:40] = cols(inp["g_out_fox"][l], 4)
        prm[l, :, 40:42] = cols(inp["g_q_lora"][l], 2)
        prm[l, :, 42] = inp["g_kv_lora"][l]
        prm[l, 0:96, 43] = inp["g_mla_q"][l]
        prm[l, 0:96, 44] = inp["g_mla_k"][l]
        prm[l, 0:64, 45] = inp["g_fox_q"][l]
        prm[l, 64:128, 45] = inp["g_fox_q"][l]
        prm[l, 0:64, 46] = inp["g_fox_k"][l]
        prm[l, 64:128, 46] = inp["g_fox_k"][l]
        prm[l, 0:8, 47] = inp["b_fox_f"][l]
    return prm


def make_in_maps(inp):
    f = lambda a: np.ascontiguousarray(a, dtype=np.float32)
    NE_ = 1 if os.environ.get("KSMALL") else 32
    shared = dict(
        prm=_prm(inp), cst=_consts(),
        w_in=f(inp["w_in"]), w_uq=f(inp["w_uq"]), w_ukv=f(inp["w_ukv"]), w_out=f(inp["w_out"]),
        w_rt=f(np.concatenate([inp["w_router_group"], inp["w_router_expert"]], axis=-1)),
        w_eg=f(inp["w_exp_gate"][:, :NE_]), w_eu=f(inp["w_exp_up"][:, :NE_]), w_ed=f(inp["w_exp_down"][:, :NE_]),
        w_pg=f(inp["w_ple_gate"]), w_pp=f(inp["w_ple_proj"]),
    )
    maps = []
    for c in range(8):
        b = c % 4
        m = dict(shared)
        m["xT"] = f(inp["x"][b].T)
        m["pT"] = f(np.transpose(inp["p"][:, b], (0, 2, 1)))
        m["pos"] = np.ascontiguousarray(inp["positions"][b][None, :], dtype=np.int32)
        maps.append(m)
    return maps


def kernel(**inputs):
    inp = {k: np.asarray(v) for k, v in inputs.items()}
    nc = build_program()
    maps = make_in_maps(inp)
    res = run_bass_kernel_spmd(nc, maps, core_ids=list(range(8)))
    out = np.stack([np.ascontiguousarray(res.results[b]["yT"].T) for b in range(4)], axis=0)
    return out.astype(np.float32)
```
